# Optimizing a Trainium2 kernel written in Bass

```python
import math
import jax, jax.numpy as jnp
from jax import lax
import numpy as np

D_MODEL = 1024
BATCH = 8
SEQ = 4096
DEPTH = 1

D_CONV = 512
CONV_K = 3
N_HEADS = 8
HEAD_DIM = 64
D_ATTN = N_HEADS * HEAD_DIM
N_IDX_HEADS = 8
IDX_DIM = 64
TOPK_MAX = 256
Q_BLOCK = 128
N_BUCKETS = 32
MAX_EXACT = 16
MAX_DISTANCE = 128
D_FF = -(-8 * D_MODEL // (3 * 256)) * 256
EPS = 1e-6

PROJ_SIZES = (
    D_CONV, D_CONV, D_CONV,
    D_ATTN, HEAD_DIM, HEAD_DIM,
    N_IDX_HEADS * IDX_DIM, IDX_DIM,
    N_IDX_HEADS,
    D_MODEL, D_MODEL,
)
N_PROJ = int(sum(PROJ_SIZES))
PROJ_SPLITS = [int(v) for v in np.cumsum(PROJ_SIZES)[:-1]]

kernel_name = "hybrid_shortconv_dsa_gated_block"


def rms_norm(x, g):
    xf = x.astype(jnp.float32)
    y = xf * lax.rsqrt(jnp.mean(xf * xf, axis=-1, keepdims=True) + EPS)
    return (y * g.astype(jnp.float32)).astype(x.dtype)


def rel_bucket(dist):
    n = jnp.maximum(dist, 0)
    large = MAX_EXACT + (jnp.log(jnp.maximum(n, 1).astype(jnp.float32) / MAX_EXACT)
                         / math.log(MAX_DISTANCE / MAX_EXACT)
                         * (N_BUCKETS - MAX_EXACT)).astype(jnp.int32)
    large = jnp.minimum(large, N_BUCKETS - 1)
    return jnp.where(n < MAX_EXACT, n, large)


def short_conv_mixer(gate_b, gate_c, x_a, conv_w):
    u = gate_c * x_a
    rhs = conv_w[:, None, :].astype(u.dtype)
    y = lax.conv_general_dilated(u, rhs, window_strides=(1,), padding=[(CONV_K - 1, 0)],
                                 dimension_numbers=("NWC", "WIO", "NWC"),
                                 feature_group_count=D_CONV)
    return gate_b * y


def dsa_attention(q, k, v, iq, ik, iw, rel_bias):
    bsz, seq_len = q.shape[0], q.shape[1]
    k_top = min(TOPK_MAX, seq_len // 4)
    n_blk = seq_len // Q_BLOCK
    iw = iw * (N_IDX_HEADS ** -0.5 * IDX_DIM ** -0.5)
    s_pos = jnp.arange(seq_len, dtype=jnp.int32)
    t_blocks = s_pos.reshape(n_blk, Q_BLOCK)

    def to_blocks(a):
        return jnp.moveaxis(a.reshape(bsz, n_blk, Q_BLOCK, *a.shape[2:]), 1, 0)

    gather = jax.vmap(lambda arr, ind: arr[ind])

    def block_fn(args):
        q_b, iq_b, iw_b, t_b = args
        dots = jnp.einsum("bthd,bsd->bths", iq_b, ik)
        score = jnp.einsum("bth,bths->bts", iw_b, jax.nn.relu(dots)).astype(jnp.float32)
        causal = s_pos[None, :] <= t_b[:, None]
        score = jnp.where(causal[None], score, -jnp.inf)
        _, idx = lax.top_k(score, k_top)
        valid = idx <= t_b[None, :, None]
        k_sel = gather(k, idx)
        v_sel = gather(v, idx)
        logits = jnp.einsum("bthd,btkd->bhtk", q_b, k_sel).astype(jnp.float32) * (HEAD_DIM ** -0.5)
        bias = rel_bias[rel_bucket(t_b[None, :, None] - idx)]
        logits = logits + jnp.moveaxis(bias, -1, 1).astype(jnp.float32)
        logits = jnp.where(valid[:, None], logits, -jnp.inf)
        p = jax.nn.softmax(logits, axis=-1).astype(v.dtype)
        o = jnp.einsum("bhtk,btkd->bthd", p, v_sel)
        return o.reshape(bsz, Q_BLOCK, D_ATTN)

    out = lax.map(block_fn, (to_blocks(q), to_blocks(iq), to_blocks(iw), t_blocks))
    return jnp.moveaxis(out, 0, 1).reshape(bsz, seq_len, D_ATTN)


def setup_inputs(seed: int = 0) -> dict:
    key = jax.random.key(seed)
    ks = jax.random.split(key, 16)
    f32 = jnp.float32
    nrm = lambda k, shape, scale: jax.random.normal(k, shape, f32) * scale
    return {
        "x": nrm(ks[0], (BATCH, SEQ, D_MODEL), 1.0),
        "g_mix": 1.0 + nrm(ks[1], (DEPTH, D_MODEL), 0.02),
        "w_in": nrm(ks[2], (DEPTH, D_MODEL, N_PROJ), D_MODEL ** -0.5),
        "b_gate": nrm(ks[3], (DEPTH, 2 * D_MODEL), 0.02),
        "conv_w": nrm(ks[4], (DEPTH, CONV_K, D_CONV), CONV_K ** -0.5),
        "w_branch_a": nrm(ks[5], (DEPTH, D_CONV, D_MODEL), D_CONV ** -0.5),
        "w_branch_b": nrm(ks[6], (DEPTH, D_ATTN, D_MODEL), D_ATTN ** -0.5),
        "w_out": nrm(ks[7], (DEPTH, D_MODEL, D_MODEL), D_MODEL ** -0.5),
        "rel_bias": nrm(ks[8], (N_BUCKETS, N_HEADS), 0.5),
        "g_ffn": 1.0 + nrm(ks[9], (DEPTH, D_MODEL), 0.02),
        "w_ffn_gate": nrm(ks[10], (DEPTH, D_MODEL, D_FF), D_MODEL ** -0.5),
        "w_ffn_up": nrm(ks[11], (DEPTH, D_MODEL, D_FF), D_MODEL ** -0.5),
        "w_ffn_down": nrm(ks[12], (DEPTH, D_FF, D_MODEL), D_FF ** -0.5),
        "g_final": 1.0 + nrm(ks[13], (D_MODEL,), 0.02),
    }


def reference(x, g_mix, w_in, b_gate, conv_w, w_branch_a, w_branch_b, w_out, rel_bias,
              g_ffn, w_ffn_gate, w_ffn_up, w_ffn_down, g_final):
    bsz, seq_len, _ = x.shape
    for layer in range(DEPTH):
        h = rms_norm(x, g_mix[layer])
        proj = h @ w_in[layer]
        (c_b, c_c, c_x, q, k, v, iq, ik, iw, g_a, g_b) = jnp.split(proj, PROJ_SPLITS, axis=-1)
        y_a = short_conv_mixer(c_b, c_c, c_x, conv_w[layer]) @ w_branch_a[layer]
        attn = dsa_attention(q.reshape(bsz, seq_len, N_HEADS, HEAD_DIM), k, v,
                             iq.reshape(bsz, seq_len, N_IDX_HEADS, IDX_DIM), ik, iw, rel_bias)
        y_b = attn @ w_branch_b[layer]
        gates = jax.nn.sigmoid(jnp.concatenate([g_a, g_b], axis=-1) + b_gate[layer])
        merged = gates[..., :D_MODEL] * y_a + gates[..., D_MODEL:] * y_b
        x = x + merged @ w_out[layer]
        h2 = rms_norm(x, g_ffn[layer])
        x = x + (jax.nn.silu(h2 @ w_ffn_gate[layer]) * (h2 @ w_ffn_up[layer])) @ w_ffn_down[layer]
    return rms_norm(x, g_final)
```

```python
import math
import numpy as np
import ml_dtypes
from contextlib import ExitStack
import concourse.bass as bass
import concourse.mybir as mybir
from concourse.bass_utils import run_bass_kernel_spmd

F32 = mybir.dt.float32
BF16 = mybir.dt.bfloat16
U8 = mybir.dt.uint8
AF = mybir.ActivationFunctionType
ALU = mybir.AluOpType
AX = mybir.AxisListType

D_MODEL = 1024
D_CONV = 512
N_HEADS = 8
HEAD_DIM = 64
D_ATTN = 512
N_IDX = 8
IDX_DIM = 64
TOPK = 256
N_BUCKETS = 32
MAX_EXACT = 16
MAX_DISTANCE = 128
D_FF = 2816
NFC = D_FF // 128
EPS = 1e-6
PROJ_SIZES = (512, 512, 512, 512, 64, 64, 512, 64, 8, 1024, 1024)
OFFS = np.concatenate([[0], np.cumsum(PROJ_SIZES)]).astype(int)
O_CB, O_CC, O_CX, O_Q, O_K, O_V, O_IQ, O_IK, O_IW, O_GA, O_GB = [int(v) for v in OFFS[:-1]]
T = 512
NEG = -30000.0
KBIS = 21
IW_SCALE = float(N_IDX ** -0.5 * IDX_DIM ** -0.5)

C_ID = 0
C_CAUS = 128
C_GF = 256
C_BG = C_GF + 1024
C_GM = C_BG + 16
C_GFFN = C_GM + 8
C_CW = C_GFFN + 8
C_BFAR = C_CW + 12
C_P2 = C_BFAR + 8
C_ONE = C_P2 + KBIS
NCST = C_ONE + 64

E_K, E_IK, E_TM = 0, 1, 2
E_Q = 3
E_IQ = 7
E_CONV = 11
E_MIX = 23
E_OUT = 55
E_GU = 63
E_DN = 107
M_ENT = 131
GRP = 2
RING = 8


class Sched:
    def __init__(self, upto=99):
        self.ops = []
        self.upto = upto
        self.on = True

    def allkeys(self):
        ks = set()
        for op in self.ops:
            ks.update(op[2]); ks.update(op[3])
        return sorted(k for k in ks if not k.startswith("outd") and not k.startswith("wbf"))

    def stage(self, n):
        self.main_on = n <= self.upto
        self.on = self.main_on

    def sub(self, v):
        self.on = self.main_on and v <= SUBUPTO

    def add(self, eng, fn, r=(), w=(), dma=False):
        if self.on:
            self.ops.append((eng, fn, tuple(r), tuple(w), dma))

    def analyze(self, sems, dma_sems):
        ops = self.ops
        n = len(ops)
        last_w, readers = {}, {}
        deps = []
        for i, (eng, fn, r, w, dma) in enumerate(ops):
            d = set()
            for b in r:
                if b in last_w:
                    d.add(last_w[b])
                if b.startswith("ps"):
                    d.update(p for p in readers.get(b, ()) if ops[p][0] != eng)
            for b in w:
                if b in last_w:
                    d.add(last_w[b])
                d.update(readers.get(b, ()))
            d.discard(i)
            for b in r:
                readers.setdefault(b, []).append(i)
            for b in w:
                last_w[b] = i
                readers[b] = []
            deps.append(d)
        red = []
        marked = set()
        for i in range(n):
            eng = ops[i][0]
            best = {}
            dl = []
            for p in deps[i]:
                pe, _, _, _, pd = ops[p]
                if pd:
                    dl.append(p)
                    continue
                if pe == eng and eng == "tensor":
                    continue
                if pe not in best or best[pe] < p:
                    best[pe] = p
            lst = list(best.values()) + dl
            red.append(lst)
            marked.update(lst)
        cnt = {}
        self.inc = {}
        dcount = {}
        self.prewait = {}
        for i in range(n):
            eng, _, _, _, dma = ops[i]
            if dma:
                k = dcount.get(eng, 0)
                dcount[eng] = k + 1
                ns = len(dma_sems[eng])
                sem = dma_sems[eng][k % ns]
                self.inc[i] = (sem, 16 * (k // ns + 1))
                if k >= ns:
                    self.prewait[i] = (sem, 16 * (k // ns))
            elif i in marked:
                cnt[eng] = cnt.get(eng, 0) + 1
                self.inc[i] = (sems[eng], cnt[eng])
        self.waits = []
        for i in range(n):
            wl = [self.inc[p] for p in red[i]]
            if i in self.prewait:
                wl.append(self.prewait[i])
            self.waits.append(wl)
        self.by_eng = {}
        for i in range(n):
            self.by_eng.setdefault(ops[i][0], []).append(i)

    def emit(self, engname, e):
        waited = {}
        for i in self.by_eng.get(engname, []):
            eng, fn, r, w, dma = self.ops[i]
            for sem, val in self.waits[i]:
                k = id(sem)
                if waited.get(k, 0) < val:
                    e.wait_ge(sem, val)
                    waited[k] = val
            inst = fn(e)
            if i in self.inc:
                sem, val = self.inc[i]
                inst.then_inc(sem, 16 if dma else 1)

    def final_waits(self, e, engname):
        pass


def build(L, kbis=KBIS, upto=99):
    NT = L // T
    NKB = L // 128
    nc = bass.Bass("TRN2", target_bir_lowering=False)
    x_d = nc.dram_tensor("x", [L, D_MODEL], F32, kind="ExternalInput").ap()
    wall_d = nc.dram_tensor("wall", [M_ENT, 128, 1024], F32, kind="ExternalInput").ap()
    cst_d = nc.dram_tensor("cst", [128, NCST], F32, kind="ExternalInput").ap()
    bias_d = nc.dram_tensor("biasT", [128, 2048], F32, kind="ExternalInput").ap()
    out_d = nc.dram_tensor("out", [L, D_MODEL], F32, kind="ExternalOutput").ap()
    wbf_d = nc.dram_tensor("wbf", [M_ENT, 128, 1024], BF16).ap()
    if DEBUG:
        d_bis = nc.dram_tensor("d_bis", [128, 64], F32, kind="ExternalOutput").ap()
        d_sc = nc.dram_tensor("d_sc", [128, 4096], F32, kind="ExternalOutput").ap()
        d_mask = nc.dram_tensor("d_mask", [128, 4096], F32, kind="ExternalOutput").ap()
        d_attn = nc.dram_tensor("d_attn", [128, 2048], F32, kind="ExternalOutput").ap()

    S = Sched(upto)
    es = ExitStack()

    def sb(name, shape, dt):
        return es.enter_context(nc.sbuf_tensor("sb_" + name, shape, dt))

    with es:
        cst = sb("cst", [128, NCST], F32)
        identb = sb("identb", [128, 128], BF16)
        bhi = sb("bhi", [128, 2048], BF16)
        blo = sb("blo", [128, 2048], BF16)
        kT = sb("kT", [128, L], BF16)
        ikT = sb("ikT", [128, L], BF16)
        vab = sb("vab", [128, NKB, 192], BF16)
        uhalo = sb("uhalo", [128, 4, 2], F32)
        wtok = sb("wtok", [128, 4, 8], F32)
        xt = sb("xt", [128, 4, 1024], F32)
        hT = sb("hT", [128, 8, T], BF16)
        yaT = sb("yaT", [128, 4, T], BF16)
        qTe = sb("qTe", [128, 4, T], BF16)
        qTo = sb("qTo", [128, 4, T], BF16)
        iqTe = sb("iqTe", [128, 4, T], BF16)
        iqTo = sb("iqTo", [128, 4, T], BF16)
        attnT = sb("attnT", [128, 4, T], BF16)
        arA = sb("arA", [128, 16384], BF16)
        arB = sb("arB", [128, 8200], F32)
        maskb = sb("maskb", [128, 4096], BF16)
        arC = sb("arC", [128, 4096], BF16)
        dtl = sb("dtl", [128, 2, 8, 128], BF16)
        rl = sb("rl", [128, 4, T], BF16)
        ring = sb("ring", [128, RING, 1024], BF16)
        sm = sb("sm", [128, 64], F32)
        bis = sb("bis", [128, 2, 32], F32)
        steps = sb("steps", [128, 2, 32], F32)
        psb = [es.enter_context(nc.psum_tensor("ps%d" % i, [128, 512], F32)) for i in range(8)]

        negm = arA[:, 0:NKB * 512].rearrange("p (k t) -> p k t", t=512)
        act = arA[:, 0:NFC * 512].rearrange("p (k t) -> p k t", t=512)
        A_KEYS = tuple("A%d" % i for i in range(32))

        def negm_keys(kb):
            return ("A%d" % kb,)

        def act_keys(fc):
            return ("A%d" % fc,)

        scores = [arB[:, 0:4096], arB[:, 4096:8192]]
        merged = arB[:, 0:2048].bitcast(BF16).rearrange("p (k t) -> p k t", t=512)
        moT = [arB[:, 2048:2560], arB[:, 2560:3072]]
        tmpa = [arB[:, 3072:3584], arB[:, 3584:4096]]
        SC0_KEYS = tuple(["mg%d" % i for i in range(8)] + ["moT0", "moT1", "tmpa0", "tmpa1"])
        sg = [arB[:, 4096:4608], arB[:, 4608:5120]]
        t2 = [arB[:, 5120:5632], arB[:, 5632:6144]]
        den_sb = arB[:, 6144:6656]
        rbc = arB[:, 6656:7168]
        cct = arB[:, 7168:7680]
        ubuf = arB[:, 7680:7680 + 514]
        yt = arB[:, 4096:4608]
        SC1_KEYS = ("sg0", "sg1", "t20", "t21", "den", "rbc", "cct", "ubuf")
        SC_KEYS = [SC0_KEYS, SC1_KEYS]
        xn = [arC[:, 0:1024], arC[:, 1024:2048]]
        PT = [arC[:, 2048 + i * 512: 2048 + (i + 1) * 512] for i in range(4)] + \
             [arC[:, i * 512:(i + 1) * 512] for i in range(4)]
        NPT = 8

        def pt_keys(pi):
            return ("PT%d" % pi,) if pi < 4 else ("PT%d" % pi, "xn%d" % ((pi - 4) // 2))
        junk = arC[:, 0:4096]

        ident_f = cst[:, C_ID:C_ID + 128]
        caus = cst[:, C_CAUS:C_CAUS + 128]

        def bank(i):
            return psb[i][:]

        def bankb(i):
            return psb[i][:].bitcast(BF16)

        S.add("sync", lambda e: e.dma_start(out=cst[:], in_=cst_d), w=("cst",), dma=True)
        S.add("sync", lambda e: e.dma_start(out=arB[:, 0:2048], in_=bias_d), w=SC0_KEYS, dma=True)
        S.add("vector", lambda e: e.tensor_copy(out=identb[:], in_=ident_f), r=("cst",), w=("identb",))
        S.add("vector", lambda e: e.tensor_copy(out=bhi[:], in_=arB[:, 0:2048]), r=SC0_KEYS, w=("bhi",))
        S.add("vector", lambda e: e.tensor_tensor(out=arB[:, 2048:4096], in0=arB[:, 0:2048], in1=bhi[:],
                                                  op=ALU.subtract), r=SC0_KEYS + ("bhi",), w=SC0_KEYS)
        S.add("vector", lambda e: e.tensor_copy(out=blo[:], in_=arB[:, 2048:4096]), r=SC0_KEYS, w=("blo",))
        S.add("vector", lambda e: e.memset(vab[:], 0.0), w=("vab",))
        for zi, zt in enumerate((qTe, qTo, iqTe, iqTo)):
            S.add("vector" if zi % 2 == 0 else "gpsimd", (lambda zt: lambda e: e.memset(zt[:], 0.0))(zt), w=("zq%d" % zi,))
        S.add("vector", lambda e: e.memset(vab[:, :, 64:65], 1.0), w=("vab",))
        S.add("vector", lambda e: e.memset(uhalo[:], 0.0), w=("uhalo",))
        CH = 8
        for a in range(0, M_ENT, CH):
            b = min(M_ENT, a + CH)
            S.add("gpsimd", (lambda a, b: lambda e: e.dma_start(out=wbf_d[a:b], in_=wall_d[a:b]))(a, b),
                  w=tuple("wbf%d" % i for i in range(a, b)), dma=True)

        NGRP = (M_ENT + GRP - 1) // GRP
        TOTG = NGRP * NT
        NSLOTG = RING // GRP
        state = {"loaded": 0}

        def load_group(g):
            j, gi = divmod(g, NGRP)
            e0 = gi * GRP
            e1 = min(M_ENT, e0 + GRP)
            s0 = (g % NSLOTG) * GRP
            n = e1 - e0
            S.add("sync", lambda e: e.dma_start(
                out=ring[:, s0:s0 + n, :], in_=wbf_d[e0:e1].rearrange("e p f -> p e f")),
                r=tuple("wbf%d" % i for i in range(e0, e1)),
                w=tuple("ring%d" % (s0 + i) for i in range(n)), dma=True)

        def want(j, ent):
            g = j * NGRP + ent // GRP
            while S.on and state["loaded"] <= min(TOTG - 1, g + NSLOTG - 1):
                load_group(state["loaded"])
                state["loaded"] += 1
            slot = (g % NSLOTG) * GRP + ent % GRP
            return ring[:, slot, :].rearrange("p (k c) -> p k c", c=128), "ring%d" % slot

        ps_rot = {"i": 0}

        def next_bank(lo=0, hi=4):
            i = ps_rot["i"]
            ps_rot["i"] = (i + 1) % 4
            return i

        def mm(out, lhsT, rhs, start, stop, r, w):
            S.add("tensor", lambda e: e.matmul(out, lhsT, rhs, start=start, stop=stop), r=r, w=w)

        def proj_fm(j, ent, nk, rhs_fn, rkeys, b):
            wap, wkey = want(j, ent)
            for kc in range(nk):
                mm(bank(b), wap[:, kc, :], rhs_fn(kc), kc == 0, kc == nk - 1,
                   r=(wkey,) + tuple(rkeys(kc)), w=("ps%d" % b,))

        def rmsnorm_to_hT(gcol, tagr):
            for s in range(4):
                S.sub(1)
                S.add("scalar", (lambda s: lambda e: e.activation(
                    out=xn[s % 2], in_=xt[:, s, :], func=AF.Square, accum_out=sm[:, s:s + 1]))(s),
                    r=("xt%d" % s,), w=("xn%d" % (s % 2), "sm%d" % s))
                S.sub(2)
                S.add("vector", (lambda s: lambda e: e.tensor_scalar(
                    out=sm[:, 4 + s:5 + s], in0=sm[:, s:s + 1], scalar1=1.0 / D_MODEL, scalar2=EPS,
                    op0=ALU.mult, op1=ALU.add))(s), r=("sm%d" % s,), w=("sm%d" % (4 + s),))
                S.sub(3)
                S.add("scalar", (lambda s: lambda e: e.activation(
                    out=sm[:, 8 + s:9 + s], in_=sm[:, 4 + s:5 + s], func=AF.Sqrt))(s),
                    r=("sm%d" % (4 + s),), w=("sm%d" % (8 + s),))
                S.add("vector", (lambda s: lambda e: e.reciprocal(
                    out=sm[:, 12 + s:13 + s], in_=sm[:, 8 + s:9 + s]))(s),
                    r=("sm%d" % (8 + s),), w=("sm%d" % (12 + s),))
                S.sub(4)
                S.add("vector", (lambda s: lambda e: e.tensor_scalar(
                    out=xn[s % 2], in0=xt[:, s, :], scalar1=sm[:, 12 + s:13 + s], scalar2=None,
                    op0=ALU.mult))(s), r=("xt%d" % s, "sm%d" % (12 + s)), w=("xn%d" % (s % 2),))
                S.sub(5)
                b = 4 + (s % 2)
                for c in range(8):
                    S.add("tensor", (lambda s, c, b: lambda e: e.transpose(
                        out=bankb(b)[:, c * 128:(c + 1) * 128], in_=xn[s % 2][:, c * 128:(c + 1) * 128],
                        identity=identb[:]))(s, c, b),
                        r=("xn%d" % (s % 2), "identb"), w=("ps%d" % b,))
                S.sub(6)
                for c in range(8):
                    eng = "vector" if c % 2 == 0 else "gpsimd"
                    if eng == "gpsimd":
                        eng = EVAC_ENG
                    if eng == "vector":
                        S.add("vector", (lambda s, c, b: lambda e: e.tensor_scalar(
                            out=hT[:, c, s * 128:(s + 1) * 128], in0=bankb(b)[:, c * 128:(c + 1) * 128],
                            scalar1=cst[:, gcol + c:gcol + c + 1], scalar2=None, op0=ALU.mult))(s, c, b),
                            r=("ps%d" % b, "cst"), w=("hT%d" % c,))
                    else:
                        S.add("scalar", (lambda s, c, b: lambda e: e.activation(
                            out=hT[:, c, s * 128:(s + 1) * 128], in_=bankb(b)[:, c * 128:(c + 1) * 128],
                            func=AF.Identity, scale=cst[:, gcol + c:gcol + c + 1]))(s, c, b),
                            r=("ps%d" % b, "cst"), w=("hT%d" % c,))

            S.sub(0)

        XT_KEYS = ("xt0", "xt1", "xt2", "xt3")
        HT_KEYS = tuple("hT%d" % c for c in range(8))

        def hT_rhs(kc):
            return hT[:, kc, :]

        def hT_keys(kc):
            return ("hT%d" % kc,)

        if DEBUG3:
            for ent in range(min(M_ENT, L // 128)):
                wap, wkey = want(0, ent)
                slot = int(wkey[4:])
                S.add("vector", (lambda slot, ent: lambda e: e.tensor_copy(
                    out=arB[:, (ent % 2) * 1024:(ent % 2) * 1024 + 1024], in_=ring[:, slot, :]))(slot, ent),
                    r=(wkey,), w=("stg%d" % (ent % 2),))
                S.add("gpsimd", (lambda ent: lambda e: e.dma_start(
                    out=out_d[128 * ent:128 * ent + 128, :], in_=arB[:, (ent % 2) * 1024:(ent % 2) * 1024 + 1024]))(ent),
                    r=("stg%d" % (ent % 2),), w=("od%d" % ent,), dma=True)
            S.add("gpsimd", lambda e: e.nop(), r=tuple("od%d" % ent for ent in range(min(M_ENT, L // 128))))
            NT = 0
        for j in range(NT):
            t0 = j * T
            S.stage(1)
            for s_ in range(4):
                S.add("gpsimd", (lambda t0, s_: lambda e: e.dma_start(
                    out=xt[:, s_, :], in_=x_d[t0 + 128 * s_:t0 + 128 * s_ + 128, :]))(t0, s_),
                    w=("xt%d" % s_,), dma=True)
            rmsnorm_to_hT(C_GM, "a")

            S.stage(2)
            S.sub(1)
            b = next_bank()
            proj_fm(j, E_K, 8, hT_rhs, hT_keys, b)
            S.add("scalar", (lambda b, t0: lambda e: e.activation(out=kT[:, t0:t0 + T], in_=bank(b), func=AF.Copy))(b, t0),
                  r=("ps%d" % b,), w=tuple("kT%d" % (4 * j + i) for i in range(4)))
            S.sub(2)
            b = next_bank()
            proj_fm(j, E_IK, 8, hT_rhs, hT_keys, b)
            S.add("vector", (lambda b, t0: lambda e: e.tensor_copy(out=ikT[:, t0:t0 + T], in_=bank(b)))(b, t0),
                  r=("ps%d" % b,), w=tuple("ikT%d" % (4 * j + i) for i in range(4)))
            S.sub(3)
            b = next_bank()
            wap, wkey = want(j, E_TM)
            for s in range(4):
                for kc in range(8):
                    mm(bank(b)[:, s * 128:s * 128 + 72], hT[:, kc, s * 128:(s + 1) * 128], wap[:, kc, 0:72],
                       kc == 0, kc == 7, r=(wkey, "hT%d" % kc), w=("ps%d" % b,))
            for s in range(4):
                S.add("scalar", (lambda b, s, kbw: lambda e: e.activation(
                    out=vab[:, kbw, 0:64], in_=bank(b)[:, s * 128:s * 128 + 64], func=AF.Copy))(b, s, 4 * j + s),
                    r=("ps%d" % b, "vab"), w=("vab%d" % (4 * j + s),))
                S.add("vector", (lambda b, s, kbw: lambda e: e.tensor_copy(
                    out=vab[:, kbw, 128:192], in_=bank(b)[:, s * 128:s * 128 + 64]))(b, s, 4 * j + s),
                    r=("ps%d" % b, "vab"), w=("vab%d" % (4 * j + s),))
                S.add("vector", (lambda b, s: lambda e: e.tensor_scalar(
                    out=wtok[:, s, :], in0=bank(b)[:, s * 128 + 64:s * 128 + 72], scalar1=IW_SCALE,
                    scalar2=None, op0=ALU.mult))(b, s), r=("ps%d" % b,), w=("wtok%d" % s,))
            S.sub(4)
            for c in range(4):
                b = next_bank()
                proj_fm(j, E_Q + c, 8, hT_rhs, hT_keys, b)
                S.add("scalar", (lambda b, c: lambda e: e.activation(
                    out=qTe[0:64, c, :], in_=bank(b)[0:64, :], func=AF.Copy, scale=HEAD_DIM ** -0.5))(b, c),
                    r=("ps%d" % b, "zq0"), w=("qT%d" % c,))
                S.add("scalar", (lambda b, c: lambda e: e.activation(
                    out=qTo[64:128, c, :], in_=bank(b)[64:128, :], func=AF.Copy, scale=HEAD_DIM ** -0.5))(b, c),
                    r=("ps%d" % b, "zq1"), w=("qT%d" % c,))
            S.sub(5)
            for c in range(4):
                b = next_bank()
                proj_fm(j, E_IQ + c, 8, hT_rhs, hT_keys, b)
                S.add("vector", (lambda b, c: lambda e: e.tensor_copy(out=iqTe[0:64, c, :], in_=bank(b)[0:64, :]))(b, c),
                      r=("ps%d" % b, "zq2"), w=("iqT%d" % c,))
                S.add("vector", (lambda b, c: lambda e: e.tensor_copy(out=iqTo[64:128, c, :], in_=bank(b)[64:128, :]))(b, c),
                      r=("ps%d" % b, "zq3"), w=("iqT%d" % c,))
            S.sub(6)
            for c in range(4):
                b = next_bank()
                proj_fm(j, E_CONV + 3 * c, 8, hT_rhs, hT_keys, b)
                S.add("scalar", (lambda b: lambda e: e.activation(out=cct, in_=bank(b), func=AF.Copy))(b),
                      r=("ps%d" % b,), w=("cct",))
                b = next_bank()
                proj_fm(j, E_CONV + 3 * c + 1, 8, hT_rhs, hT_keys, b)
                S.add("gpsimd", (lambda c: lambda e: e.tensor_copy(out=ubuf[:, 0:2], in_=uhalo[:, c, :]))(c),
                      r=("uhalo",), w=("ubuf",))
                S.add("vector", (lambda b: lambda e: e.tensor_tensor(
                    out=ubuf[:, 2:514], in0=bank(b), in1=cct, op=ALU.mult))(b),
                    r=("ps%d" % b, "cct"), w=("ubuf",))
                S.add("gpsimd", (lambda c: lambda e: e.tensor_copy(out=uhalo[:, c, :], in_=ubuf[:, 512:514]))(c),
                      r=("ubuf",), w=("uhalo",))
                cw = C_CW + 3 * c
                S.add("vector", (lambda cw: lambda e: e.tensor_scalar(
                    out=yt, in0=ubuf[:, 2:514], scalar1=cst[:, cw + 2:cw + 3], scalar2=None, op0=ALU.mult))(cw),
                    r=("ubuf", "cst"), w=("sg0",))
                S.add("vector", (lambda cw: lambda e: e.scalar_tensor_tensor(
                    out=yt, in0=ubuf[:, 1:513], scalar=cst[:, cw + 1:cw + 2], in1=yt,
                    op0=ALU.mult, op1=ALU.add))(cw), r=("ubuf", "cst", "sg0"), w=("sg0",))
                S.add("vector", (lambda cw: lambda e: e.scalar_tensor_tensor(
                    out=yt, in0=ubuf[:, 0:512], scalar=cst[:, cw:cw + 1], in1=yt,
                    op0=ALU.mult, op1=ALU.add))(cw), r=("ubuf", "cst", "sg0"), w=("sg0",))
                b = next_bank()
                proj_fm(j, E_CONV + 3 * c + 2, 8, hT_rhs, hT_keys, b)
                S.add("vector", (lambda b, c: lambda e: e.tensor_tensor(
                    out=yaT[:, c, :], in0=bank(b), in1=yt, op=ALU.mult))(b, c),
                    r=("ps%d" % b, "sg0"), w=("yaT%d" % c,))

            S.stage(3)
            for pair in range(2):
                blks = [2 * pair, 2 * pair + 1]
                for slot, tl in enumerate(blks):
                    tb = 4 * j + tl
                    n_s = (tb + 1) * 128
                    sck = SC_KEYS[slot]
                    for h in range(8):
                        S.add("vector", (lambda slot, h, tl: lambda e: e.tensor_scalar(
                            out=dtl[:, slot, h, :], in0=identb[:], scalar1=wtok[:, tl, h:h + 1], scalar2=None,
                            op0=ALU.mult))(slot, h, tl), r=("identb", "wtok%d" % tl), w=("dtl%d_%d" % (slot, h),))
                    nsc = (n_s + 511) // 512
                    for sc in range(nsc):
                        wd = min(512, n_s - sc * 512)
                        bs = 6 + (sc % 2)
                        dbk = {}

                        def idx_s1(h):
                            c, hf = h // 2, h % 2
                            bd = next_bank()
                            dbk[h] = bd
                            S.add("tensor", (lambda bd, c, hf, tl, sc, wd: lambda e: e.matmul(
                                bank(bd)[:, 0:wd], (iqTe if hf == 0 else iqTo)[:, c, tl * 128:(tl + 1) * 128],
                                ikT[:, sc * 512:sc * 512 + wd], start=True, stop=True))(bd, c, hf, tl, sc, wd),
                                r=("iqT%d" % c,) + tuple("ikT%d" % (4 * sc + i) for i in range((wd + 127) // 128)),
                                w=("ps%d" % bd,))
                            ri = h % 4
                            if h % 2 == 0:
                                S.add("scalar", (lambda bd, ri, wd: lambda e: e.activation(
                                    out=rl[:, ri, 0:wd], in_=bank(bd)[:, 0:wd], func=AF.Relu))(bd, ri, wd),
                                    r=("ps%d" % bd,), w=("rl%d" % ri,))
                            else:
                                S.add("vector", (lambda bd, ri, wd: lambda e: e.tensor_scalar(
                                    out=rl[:, ri, 0:wd], in0=bank(bd)[:, 0:wd], scalar1=0.0, scalar2=None,
                                    op0=ALU.max))(bd, ri, wd), r=("ps%d" % bd,), w=("rl%d" % ri,))

                        def idx_s3(h):
                            ri = h % 4
                            S.add("tensor", (lambda bs, slot, h, ri, wd: lambda e: e.matmul(
                                bank(bs)[:, 0:wd], dtl[:, slot, h, :], rl[:, ri, 0:wd], start=(h == 0), stop=(h == 7)))(bs, slot, h, ri, wd),
                                r=("dtl%d_%d" % (slot, h), "rl%d" % ri), w=("ps%d" % bs,))

                        for hh in range(8 + 2):
                            if hh < 8:
                                idx_s1(hh)
                            if hh >= 2:
                                idx_s3(hh - 2)
                        S.add("vector" if sc % 2 == 0 else "scalar",
                              (lambda bs, slot, sc, wd: (lambda e: e.tensor_copy(
                                  out=scores[slot][:, sc * 512:sc * 512 + wd], in_=bank(bs)[:, 0:wd])) if sc % 2 == 0 else
                               (lambda e: e.activation(out=scores[slot][:, sc * 512:sc * 512 + wd],
                                                       in_=bank(bs)[:, 0:wd], func=AF.Copy)))(bs, slot, sc, wd),
                              r=("ps%d" % bs,), w=sck)
                    S.add("vector", (lambda slot, n_s: lambda e: e.tensor_reduce(
                        out=bis[:, slot, 0:1], in_=scores[slot][:, 0:n_s], op=ALU.max, axis=AX.X))(slot, n_s),
                        r=sck, w=("bis%d_0" % slot,))
                    S.add("vector", (lambda slot: lambda e: e.tensor_scalar(
                        out=bis[:, slot, 1:2], in0=bis[:, slot, 0:1], scalar1=-31.0, scalar2=None, op0=ALU.add))(slot),
                        r=sck + ("bis%d_0" % slot,), w=("bis%d_1" % slot,))
                    S.add("vector", (lambda slot: lambda e: e.tensor_scalar(
                        out=bis[:, slot, 1:2], in0=bis[:, slot, 1:2], scalar1=-1.0, scalar2=None, op0=ALU.add))(slot),
                        r=("bis%d_1" % slot,), w=("bis%d_1" % slot,))
                    S.add("gpsimd", (lambda slot, n_s: lambda e: e.tensor_tensor(
                        out=scores[slot][:, n_s - 128:n_s], in0=scores[slot][:, n_s - 128:n_s], in1=caus,
                        op=ALU.add))(slot, n_s), r=sck + ("cst", "bis%d_0" % slot, "bis%d_1" % slot), w=sck)
                    S.add("gpsimd", (lambda slot: lambda e: e.tensor_tensor(
                        out=bis[:, slot, 2:3], in0=bis[:, slot, 0:1], in1=bis[:, slot, 1:2], op=ALU.subtract))(slot),
                        r=("bis%d_0" % slot, "bis%d_1" % slot), w=("bis%d_2" % slot,))
                    S.add("gpsimd", (lambda slot: lambda e: e.tensor_scalar(
                        out=steps[:, slot, 0:kbis], in0=cst[:, C_P2:C_P2 + kbis], scalar1=bis[:, slot, 2:3],
                        scalar2=None, op0=ALU.mult))(slot), r=("cst", "bis%d_2" % slot), w=("steps%d" % slot,))
                    S.add("gpsimd", (lambda slot: lambda e: e.tensor_tensor(
                        out=bis[:, slot, 3:4], in0=bis[:, slot, 1:2], in1=steps[:, slot, 0:1], op=ALU.add))(slot),
                        r=("bis%d_1" % slot, "steps%d" % slot), w=("bis%d_3" % slot,))
                junkV = arC[:, 0:4096].bitcast(U8)[:, 0:4096]
                junkA = arC[:, 0:4096].bitcast(U8)[:, 4096:8192]
                for k in range(kbis):
                    for slot, tl in enumerate(blks):
                        n_s = (4 * j + tl + 1) * 128
                        if slot == 0:
                            S.add("vector", (lambda slot, n_s: lambda e: e.tensor_scalar(
                                out=junkV[:, 0:n_s], in0=scores[slot][:, 0:n_s], scalar1=bis[:, slot, 3:4], scalar2=None,
                                op0=ALU.is_ge, op1=ALU.add, accum_out=bis[:, slot, 4:5]))(slot, n_s),
                                r=SC_KEYS[slot] + ("bis%d_3" % slot,), w=("bis%d_4" % slot,))
                        else:
                            S.add("scalar", (lambda slot, n_s: lambda e: e.activation(
                                out=junkA[:, 0:n_s], in_=scores[slot][:, 0:n_s], func=AF.Sign, scale=-1.0,
                                bias=bis[:, slot, 3:4], accum_out=bis[:, slot, 4:5]))(slot, n_s),
                                r=SC_KEYS[slot] + ("bis%d_3" % slot,), w=("bis%d_4" % slot,))
                    for slot, tl in enumerate(blks):
                        n_s = (4 * j + tl + 1) * 128
                        if slot == 0:
                            S.add("gpsimd", (lambda slot: lambda e: e.tensor_scalar(
                                out=bis[:, slot, 5:6], in0=bis[:, slot, 4:5], scalar1=float(TOPK) - 0.5, scalar2=-0.5,
                                op0=ALU.is_ge, op1=ALU.add))(slot), r=("bis%d_4" % slot,), w=("bis%d_5" % slot,))
                        else:
                            S.add("gpsimd", (lambda slot, n_s: lambda e: e.tensor_scalar(
                                out=bis[:, slot, 5:6], in0=bis[:, slot, 4:5], scalar1=float(n_s - 2 * TOPK + 1), scalar2=-0.5,
                                op0=ALU.is_le, op1=ALU.add))(slot, n_s), r=("bis%d_4" % slot,), w=("bis%d_5" % slot,))
                        if k < kbis - 1:
                            S.add("gpsimd", (lambda slot, k: lambda e: e.tensor_scalar(
                                out=bis[:, slot, 3:4], in0=bis[:, slot, 5:6], scalar1=steps[:, slot, k:k + 1],
                                scalar2=bis[:, slot, 3:4], op0=ALU.mult, op1=ALU.add))(slot, k),
                                r=("bis%d_5" % slot, "steps%d" % slot, "bis%d_3" % slot), w=("bis%d_3" % slot,))
                        else:
                            S.add("gpsimd", (lambda slot: lambda e: e.tensor_scalar(
                                out=bis[:, slot, 5:6], in0=bis[:, slot, 5:6], scalar1=-0.5, scalar2=None,
                                op0=ALU.add))(slot), r=("bis%d_5" % slot,), w=("bis%d_5" % slot,))
                            S.add("gpsimd", (lambda slot, k: lambda e: e.tensor_scalar(
                                out=bis[:, slot, 3:4], in0=bis[:, slot, 5:6], scalar1=steps[:, slot, k:k + 1],
                                scalar2=bis[:, slot, 3:4], op0=ALU.mult, op1=ALU.add))(slot, k),
                                r=("bis%d_5" % slot, "steps%d" % slot, "bis%d_3" % slot), w=("bis%d_3" % slot,))
                for slot, tl in enumerate(blks):
                    tb = 4 * j + tl
                    n_s = (tb + 1) * 128
                    S.add("vector", (lambda slot, n_s: lambda e: e.tensor_scalar(
                        out=maskb[:, 0:n_s], in0=scores[slot][:, 0:n_s], scalar1=bis[:, slot, 3:4], scalar2=None,
                        op0=ALU.is_ge))(slot, n_s), r=SC_KEYS[slot] + ("bis%d_3" % slot,), w=("maskb",))
                    for g0 in range(0, tb + 1, 8):
                        g1 = min(tb + 1, g0 + 8)
                        bt = 4 + ((g0 // 8) % 2)
                        for kb in range(g0, g1):
                            S.add("tensor", (lambda bt, kb, g0: lambda e: e.transpose(
                                out=bankb(bt)[:, (kb - g0) * 128:(kb - g0 + 1) * 128],
                                in_=maskb[:, kb * 128:(kb + 1) * 128], identity=identb[:]))(bt, kb, g0),
                                r=("maskb", "identb"), w=("ps%d" % bt,))
                        ng = g1 - g0
                        eng = "vector" if (g0 // 8) % 2 == 0 else "gpsimd"
                        eng = "vector"
                        S.add(eng, (lambda bt, g0, ng, tl: lambda e: e.tensor_scalar(
                            out=negm[:, g0:g0 + ng, tl * 128:(tl + 1) * 128],
                            in0=bankb(bt)[:, 0:ng * 128].rearrange("p (k t) -> p k t", t=128),
                            scalar1=-1.0, scalar2=-NEG, op0=ALU.add, op1=ALU.mult))(bt, g0, ng, tl),
                            r=("ps%d" % bt,), w=tuple("A%d" % kb for kb in range(g0, g1)))

            S.stage(4)
            nkb_j = 4 * j + 4
            for h in range(8):
                c, hf = h // 2, h % 2
                bacc = 6 + (h % 2)
                bden = 4 + (h % 2)
                def att_setup(kb):
                    a = kb - 4 * j
                    col0 = max(0, a) * 128
                    near = []
                    for which in (0, 1):
                        tl = kb + which - 4 * j
                        if 0 <= tl < 4:
                            near.append((tl, which))
                    return a, col0, near

                lgb = {}

                def att_s1(kb):
                    a, col0, near = att_setup(kb)
                    bl = next_bank()
                    lgb[kb] = bl
                    S.add("tensor", (lambda bl, hf, c, kb, col0: lambda e: e.matmul(
                        bank(bl)[:, col0:512], kT[:, kb * 128:(kb + 1) * 128],
                        (qTe if hf == 0 else qTo)[:, c, col0:512], start=True, stop=False))(bl, hf, c, kb, col0),
                        r=("kT%d" % kb, "qT%d" % c), w=("ps%d" % bl,))
                    if not NOMASK:
                      S.add("tensor", (lambda bl, kb, col0, last: lambda e: e.matmul(
                        bank(bl)[:, col0:512], identb[:], negm[:, kb, col0:512], start=False, stop=last))(bl, kb, col0, len(near) == 0),
                        r=("identb", "A%d" % kb), w=("ps%d" % bl,))
                    for ni, (tl, which) in enumerate(near):
                        bc0 = (h * 2 + which) * 128
                        for part, bt_ in enumerate((bhi, blo)):
                            last = (ni == len(near) - 1) and part == 1
                            S.add("tensor", (lambda bl, tl, bt_, bc0, last: lambda e: e.matmul(
                                bank(bl)[:, tl * 128:(tl + 1) * 128], identb[:], bt_[:, bc0:bc0 + 128],
                                start=False, stop=last))(bl, tl, bt_, bc0, last),
                                r=("identb", "bhi", "blo"), w=("ps%d" % bl,))

                def att_s2(kb):
                    a, col0, near = att_setup(kb)
                    bl = lgb[kb]
                    pi = kb % NPT
                    ncols = sorted(tl for tl, _ in near)
                    nlo = ncols[0] * 128 if ncols else 512
                    nhi = (ncols[-1] + 1) * 128 if ncols else 512
                    if ncols:
                        S.add("scalar", (lambda bl, pi, nlo, nhi: lambda e: e.activation(
                            out=PT[pi][:, nlo:nhi], in_=bank(bl)[:, nlo:nhi], func=AF.Exp))(bl, pi, nlo, nhi),
                            r=("ps%d" % bl,), w=pt_keys(pi))
                    if not ncols:
                        nhi = col0
                    if nhi < 512:
                        S.add("scalar", (lambda bl, pi, nhi, h: lambda e: e.activation(
                            out=PT[pi][:, nhi:512], in_=bank(bl)[:, nhi:512], func=AF.Exp,
                            bias=cst[:, C_BFAR + h:C_BFAR + h + 1]))(bl, pi, nhi, h),
                            r=("ps%d" % bl, "cst"), w=pt_keys(pi))

                def att_s3(kb):
                    a, col0, near = att_setup(kb)
                    pi = kb % NPT
                    vlo = 0 if hf == 0 else 64
                    S.add("tensor", (lambda bacc, kb, pi, col0, vlo, lastkb: lambda e: e.matmul(
                        bank(bacc)[:, col0:512], vab[:, kb, vlo:vlo + 128], PT[pi][:, col0:512],
                        start=(kb == 0), stop=lastkb))(bacc, kb, pi, col0, vlo, kb == nkb_j - 1),
                        r=("vab%d" % kb, "vab") + pt_keys(pi), w=("ps%d" % bacc,))

                SK = 3
                for kk in range(nkb_j + SK):
                    if kk < nkb_j:
                        att_s1(kk)
                        att_s2(kk)
                    if kk >= SK:
                        att_s3(kk - SK)
                dp = 64 if hf == 0 else 0
                op0 = 0 if hf == 0 else 64
                S.add("vector", (lambda bacc, dp: lambda e: e.tensor_copy(
                    out=den_sb[dp:dp + 1, :], in_=bank(bacc)[dp:dp + 1, :]))(bacc, dp),
                    r=("ps%d" % bacc,), w=("den",))
                S.add("vector", (lambda dp: lambda e: e.reciprocal(out=den_sb[dp:dp + 1, :], in_=den_sb[dp:dp + 1, :]))(dp),
                      r=("den",), w=("den",))
                S.add("tensor", (lambda bden, dp, op0: lambda e: e.matmul(
                    bank(bden)[op0:op0 + 64, :], cst[dp:dp + 1, C_ONE:C_ONE + 64], den_sb[dp:dp + 1, :],
                    start=True, stop=True))(bden, dp, op0), r=("cst", "den"), w=("ps%d" % bden,))
                S.add("scalar", (lambda bden, op0: lambda e: e.activation(
                    out=rbc[op0:op0 + 64, :], in_=bank(bden)[op0:op0 + 64, :], func=AF.Copy))(bden, op0),
                    r=("ps%d" % bden,), w=("rbc",))
                S.add("vector", (lambda bacc, op0, c: lambda e: e.tensor_tensor(
                    out=attnT[op0:op0 + 64, c, :], in0=bank(bacc)[op0:op0 + 64, :], in1=rbc[op0:op0 + 64, :],
                    op=ALU.mult))(bacc, op0, c), r=("ps%d" % bacc, "rbc"), w=("attnT%d_%d" % (c, hf),))

            S.stage(5)
            for dc in range(8):
                e0 = E_MIX + 4 * dc
                ba = next_bank()
                proj_fm(j, e0, 4, lambda kc: yaT[:, kc, :], lambda kc: ("yaT%d" % kc,), ba)
                bg = next_bank()
                proj_fm(j, e0 + 1, 8, hT_rhs, hT_keys, bg)
                i2 = dc % 2
                S.add("scalar", (lambda bg, i2, dc: lambda e: e.activation(
                    out=sg[i2], in_=bank(bg), func=AF.Sigmoid, bias=cst[:, C_BG + dc:C_BG + dc + 1]))(bg, i2, dc),
                    r=("ps%d" % bg, "cst"), w=("sg%d" % i2,))
                S.add("vector", (lambda ba, i2: lambda e: e.tensor_tensor(
                    out=tmpa[i2], in0=bank(ba), in1=sg[i2], op=ALU.mult))(ba, i2),
                    r=("ps%d" % ba, "sg%d" % i2), w=("tmpa%d" % i2,))
                bb_ = next_bank()
                proj_fm(j, e0 + 2, 4, lambda kc: attnT[:, kc, :],
                        lambda kc: ("attnT%d_0" % kc, "attnT%d_1" % kc), bb_)
                bg2 = next_bank()
                proj_fm(j, e0 + 3, 8, hT_rhs, hT_keys, bg2)
                S.add("scalar", (lambda bg2, i2, dc: lambda e: e.activation(
                    out=t2[i2], in_=bank(bg2), func=AF.Sigmoid, bias=cst[:, C_BG + 8 + dc:C_BG + 9 + dc]))(bg2, i2, dc),
                    r=("ps%d" % bg2, "cst"), w=("t2%d" % i2,))
                S.add("vector", (lambda bb_, i2: lambda e: e.tensor_tensor(
                    out=t2[i2], in0=bank(bb_), in1=t2[i2], op=ALU.mult))(bb_, i2),
                    r=("ps%d" % bb_, "t2%d" % i2), w=("t2%d" % i2,))
                S.add("gpsimd", (lambda i2, dc: lambda e: e.tensor_tensor(
                    out=merged[:, dc, :], in0=t2[i2], in1=tmpa[i2], op=ALU.add))(i2, dc),
                    r=("t2%d" % i2, "tmpa%d" % i2), w=("mg%d" % dc,))

            def out_to_resid(bo, dc, i2):
                S.add("scalar", (lambda bo, i2: lambda e: e.activation(out=moT[i2], in_=bank(bo), func=AF.Copy))(bo, i2),
                      r=("ps%d" % bo,), w=("moT%d" % i2,))
                bt = 4 + (dc % 2)
                for s in range(4):
                    S.add("tensor", (lambda bt, s, i2: lambda e: e.transpose(
                        out=bank(bt)[:, s * 128:(s + 1) * 128], in_=moT[i2][:, s * 128:(s + 1) * 128],
                        identity=ident_f))(bt, s, i2), r=("moT%d" % i2, "cst"), w=("ps%d" % bt,))
                S.add("vector", (lambda bt, dc: lambda e: e.tensor_tensor(
                    out=xt[:, :, dc * 128:(dc + 1) * 128],
                    in0=bank(bt).rearrange("p (s d) -> p s d", d=128),
                    in1=xt[:, :, dc * 128:(dc + 1) * 128], op=ALU.add))(bt, dc),
                    r=("ps%d" % bt,) + XT_KEYS, w=XT_KEYS)

            for dc in range(8):
                bo = next_bank()
                proj_fm(j, E_OUT + dc, 8, lambda kc: merged[:, kc, :], lambda kc: ("mg%d" % kc,), bo)
                out_to_resid(bo, dc, dc % 2)

            S.stage(6)
            rmsnorm_to_hT(C_GFFN, "b")
            for fc in range(NFC):
                bg = next_bank()
                proj_fm(j, E_GU + 2 * fc, 8, hT_rhs, hT_keys, bg)
                bu = next_bank()
                proj_fm(j, E_GU + 2 * fc + 1, 8, hT_rhs, hT_keys, bu)
                i2 = fc % 2
                S.add("scalar", (lambda bg, i2: lambda e: e.activation(out=sg[i2], in_=bank(bg), func=AF.Silu))(bg, i2),
                      r=("ps%d" % bg,), w=("sg%d" % i2,))
                S.add("vector", (lambda bu, i2, fc: lambda e: e.tensor_tensor(
                    out=act[:, fc, :], in0=bank(bu), in1=sg[i2], op=ALU.mult))(bu, i2, fc),
                    r=("ps%d" % bu, "sg%d" % i2), w=("A%d" % fc,))
            for dc in range(8):
                bo = next_bank()
                for part in range(3):
                    wap, wkey = want(j, E_DN + 3 * dc + part)
                    nk = 8 if part < 2 else NFC - 16
                    for ko in range(nk):
                        fo = part * 8 + ko
                        mm(bank(bo), wap[:, ko, :], act[:, fo, :], fo == 0, fo == NFC - 1,
                           r=(wkey, "A%d" % fo), w=("ps%d" % bo,))
                out_to_resid(bo, dc, dc % 2)
            S.stage(1)
            for s in range(4):
                S.add("scalar", (lambda s: lambda e: e.activation(
                    out=xn[s % 2], in_=xt[:, s, :], func=AF.Square, accum_out=sm[:, s:s + 1]))(s),
                    r=("xt%d" % s,), w=("xn%d" % (s % 2), "sm%d" % s))
                S.add("vector", (lambda s: lambda e: e.tensor_scalar(
                    out=sm[:, 4 + s:5 + s], in0=sm[:, s:s + 1], scalar1=1.0 / D_MODEL, scalar2=EPS,
                    op0=ALU.mult, op1=ALU.add))(s), r=("sm%d" % s,), w=("sm%d" % (4 + s),))
                S.add("scalar", (lambda s: lambda e: e.activation(
                    out=sm[:, 8 + s:9 + s], in_=sm[:, 4 + s:5 + s], func=AF.Sqrt))(s),
                    r=("sm%d" % (4 + s),), w=("sm%d" % (8 + s),))
                S.add("vector", (lambda s: lambda e: e.reciprocal(
                    out=sm[:, 12 + s:13 + s], in_=sm[:, 8 + s:9 + s]))(s),
                    r=("sm%d" % (8 + s),), w=("sm%d" % (12 + s),))
                S.add("vector", (lambda s: lambda e: e.scalar_tensor_tensor(
                    out=xt[:, s, :], in0=xt[:, s, :], scalar=sm[:, 12 + s:13 + s], in1=cst[:, C_GF:C_GF + 1024],
                    op0=ALU.mult, op1=ALU.mult))(s), r=("xt%d" % s, "sm%d" % (12 + s), "cst"), w=("xt%d" % s,))
            for s_ in range(4):
                S.add("gpsimd", (lambda t0, s_: lambda e: e.dma_start(
                    out=out_d[t0 + 128 * s_:t0 + 128 * s_ + 128, :], in_=xt[:, s_, :]))(t0, s_),
                    r=("xt%d" % s_,), w=("outd%d_%d" % (j, s_),), dma=True)

        S.stage(0)
        if DEBUG2:
            lastk = ("outd%d" % (NT - 1),)
            allk = tuple(S.allkeys())
            if DBGSET == 0:
                items = [
                    [(arB[:, 4096:5120], 1024)],
                    [(iqT[:, 0, :], 512), (iqT[:, 1, :], 512)],
                    [(iqT[:, 2, :], 512), (iqT[:, 3, :], 512)],
                    [(ikT[:, 0:1024], 1024)],
                    [(wtok[:].rearrange("p a b -> p (a b)"), 32), (bis[:].rearrange("p a b -> p (a b)"), 64),
                     (dtl[:, 1, 0, :], 128), (dtl[:, 1, 1, :], 128), (rl[:, 3, :], 512)],
                    [(maskb[:, 0:1024], 1024)],
                ]
                stg = arA[:, 0:16384].bitcast(F32)
                stgk = A_KEYS
            else:
                items = [
                    [(attnT[:, 0, :], 512), (attnT[:, 1, :], 512)],
                    [(attnT[:, 2, :], 512), (attnT[:, 3, :], 512)],
                    [(negm[:, 7, :], 512), (negm[:, 0, :], 512)],
                    [(negm[:, 4, :], 512), (negm[:, 3, :], 512)],
                    [(kT[:, 0:1024], 1024)],
                    [(qT[:, 0, :], 512), (qT[:, 1, :], 512)],
                ]
                if DBGSET == 2:
                    items = [
                        [(PT[0], 512), (PT[1], 512)],
                        [(PT[2], 512), (PT[3], 512)],
                        [(bank(7), 512), (bank(5), 512)],
                        [(attnT[:, 3, :], 512), (rbc, 512)],
                        [(bank(6), 512), (bank(4), 512)],
                        [(qT[:, 3, :], 512), (kT[:, 512:1024], 512)],
                    ]
                if DBGSET == 3:
                    vflat = vab[:].rearrange("p a b -> p (a b)")
                    items = [[(vflat[:, 0:1024], 1024)], [(vflat[:, 1024:1536], 512)]]
                stg = arB[:, 0:6144] if DBGSET == 1 else arA[:, 0:16384].bitcast(F32)
                stgk = SC0_KEYS + SC1_KEYS if DBGSET == 1 else A_KEYS
            for k, lst in enumerate(items):
                c0 = 0
                for ap, n in lst:
                    S.add("vector", (lambda ap, n, k, c0: lambda e: e.tensor_copy(
                        out=stg[:, k * 1024 + c0:k * 1024 + c0 + n], in_=ap))(ap, n, k, c0),
                        r=allk, w=stgk)
                    c0 += n
                S.add("sync", (lambda k: lambda e: e.dma_start(
                    out=out_d[128 * k:128 * k + 128, :], in_=stg[:, k * 1024:(k + 1) * 1024]))(k),
                    r=stgk + lastk, w=("outd0", "outd1"), dma=True)
        if DEBUG:
            S.add("gpsimd", lambda e: e.dma_start(out=d_bis, in_=bis[:].rearrange("p a b -> p (a b)")),
                  r=tuple("bis1_%d" % i for i in range(6)), w=("dd0",), dma=True)
            S.add("gpsimd", lambda e: e.dma_start(out=d_sc, in_=arB[:, 4096:8192]), r=SC1_KEYS, w=("dd1",), dma=True)
            S.add("gpsimd", lambda e: e.dma_start(out=d_mask, in_=maskb[:], max_dma_last_dim=1024), r=("maskb",), w=("dd2",), dma=True)
            S.add("gpsimd", lambda e: e.dma_start(out=d_attn, in_=attnT[:].rearrange("p a b -> p (a b)"), max_dma_last_dim=1024),
                  r=tuple("attnT%d_%d" % (c, hf) for c in range(4) for hf in range(2)), w=("dd3",), dma=True)
            S.add("gpsimd", lambda e: e.nop(), r=("dd0", "dd1", "dd2", "dd3"))
        for j in range(NT):
            pass
        S.add("gpsimd", lambda e: e.nop(), r=tuple("outd%d_%d" % (j, s_) for j in range(NT) for s_ in range(4)))

        sem_names = ["sync", "tensor", "vector", "scalar", "gpsimd"]
        sems = {n: es.enter_context(nc.semaphore("s_" + n)) for n in sem_names}
        dma_sems = {
            "sync": [es.enter_context(nc.semaphore("ds%d" % i)) for i in range(8)],
            "gpsimd": [es.enter_context(nc.semaphore("dg%d" % i)) for i in range(8)],
        }
        S.analyze(sems, dma_sems)
        with nc.Block() as block:
            @block.sync
            def _(e):
                S.emit("sync", e)

            @block.tensor
            def _(e):
                S.emit("tensor", e)

            @block.vector
            def _(e):
                S.emit("vector", e)

            @block.scalar
            def _(e):
                S.emit("scalar", e)

            @block.gpsimd
            def _(e):
                S.emit("gpsimd", e)
    return nc


def _rel_bucket(n):
    n = np.maximum(n, 0)
    large = MAX_EXACT + (np.log(np.maximum(n, 1).astype(np.float32) / MAX_EXACT)
                         / math.log(MAX_DISTANCE / MAX_EXACT) * (N_BUCKETS - MAX_EXACT)).astype(np.int32)
    large = np.minimum(large, N_BUCKETS - 1)
    return np.where(n < MAX_EXACT, n, large)


def _entry(wcols, nk):
    ncol = wcols.shape[1]
    e = np.zeros((128, 8, 128), np.float32)
    e[:, :nk, :ncol] = wcols.reshape(nk, 128, ncol).transpose(1, 0, 2)
    return e.reshape(128, 1024)


def host_layout(w_in, b_gate, conv_w, w_a, w_b, w_out, rel_bias, g_mix, g_ffn, w_g, w_u, w_d, g_final, kbis=KBIS):
    wall = np.zeros((M_ENT, 128, 1024), np.float32)
    W = w_in

    def cols(o, n):
        return W[:, o:o + n]

    wall[E_K] = _entry(np.concatenate([cols(O_K, 64), cols(O_K, 64)], axis=1), 8)
    wall[E_IK] = _entry(np.concatenate([cols(O_IK, 64), cols(O_IK, 64)], axis=1), 8)
    wall[E_TM] = _entry(np.concatenate([cols(O_V, 64), cols(O_IW, 8)], axis=1), 8)
    for c in range(4):
        wall[E_Q + c] = _entry(cols(O_Q + 128 * c, 128), 8)
        wall[E_IQ + c] = _entry(cols(O_IQ + 128 * c, 128), 8)
        wall[E_CONV + 3 * c] = _entry(cols(O_CC + 128 * c, 128), 8)
        wall[E_CONV + 3 * c + 1] = _entry(cols(O_CX + 128 * c, 128), 8)
        wall[E_CONV + 3 * c + 2] = _entry(cols(O_CB + 128 * c, 128), 8)
    for dc in range(8):
        sl = slice(128 * dc, 128 * dc + 128)
        wall[E_MIX + 4 * dc] = _entry(w_a[:, sl], 4)
        wall[E_MIX + 4 * dc + 1] = _entry(cols(O_GA + 128 * dc, 128), 8)
        wall[E_MIX + 4 * dc + 2] = _entry(w_b[:, sl], 4)
        wall[E_MIX + 4 * dc + 3] = _entry(cols(O_GB + 128 * dc, 128), 8)
        wall[E_OUT + dc] = _entry(w_out[:, sl], 8)
        wall[E_DN + 3 * dc] = _entry(w_d[0:1024, sl], 8)
        wall[E_DN + 3 * dc + 1] = _entry(w_d[1024:2048, sl], 8)
        wall[E_DN + 3 * dc + 2] = _entry(w_d[2048:2816, sl], 6)
    for fc in range(NFC):
        sl = slice(128 * fc, 128 * fc + 128)
        wall[E_GU + 2 * fc] = _entry(w_g[:, sl], 8)
        wall[E_GU + 2 * fc + 1] = _entry(w_u[:, sl], 8)

    cst = np.zeros((128, NCST), np.float32)
    cst[:, C_ID:C_ID + 128] = np.eye(128, dtype=np.float32)
    tt, ss = np.meshgrid(np.arange(128), np.arange(128), indexing="ij")
    cst[:, C_CAUS:C_CAUS + 128] = np.where(ss <= tt, 0.0, -1e30).astype(np.float32)
    cst[:, C_GF:C_GF + 1024] = g_final[None, :]
    cst[:, C_BG:C_BG + 16] = b_gate.reshape(16, 128).T
    cst[:, C_GM:C_GM + 8] = g_mix.reshape(8, 128).T
    cst[:, C_GFFN:C_GFFN + 8] = g_ffn.reshape(8, 128).T
    for c in range(4):
        for k in range(3):
            cst[:, C_CW + 3 * c + k] = conv_w[k, 128 * c:128 * c + 128]
    cst[:, C_BFAR:C_BFAR + 8] = rel_bias[N_BUCKETS - 1][None, :]
    cst[:, C_P2:C_P2 + kbis] = (0.5 ** np.arange(1, kbis + 1)).astype(np.float32)[None, :]
    cst[:, C_ONE:C_ONE + 64] = 1.0
    sl_, tl_ = np.meshgrid(np.arange(128), np.arange(128), indexing="ij")
    biasT = np.zeros((128, 8, 2, 128), np.float32)
    for which in (0, 1):
        bk = _rel_bucket(tl_ - sl_ + 128 * which)
        biasT[:, :, which, :] = rel_bias[bk].transpose(0, 2, 1)
    return wall, cst, biasT.reshape(128, 2048)


_NC_CACHE = {}
_UPTO = 99
SUBUPTO = 99
DEBUG = False
DEBUG2 = False
DEBUG3 = False
DBGSET = 0
NOMASK = False
EVAC_ENG = 'vector'


def kernel(x, g_mix, w_in, b_gate, conv_w, w_branch_a, w_branch_b, w_out, rel_bias,
           g_ffn, w_ffn_gate, w_ffn_up, w_ffn_down, g_final):
    x = np.asarray(x, np.float32)
    B, L, _ = x.shape
    f = lambda a: np.asarray(a, np.float32)
    wall, cst, biasT = host_layout(f(w_in)[0], f(b_gate)[0], f(conv_w)[0], f(w_branch_a)[0], f(w_branch_b)[0],
                                   f(w_out)[0], f(rel_bias), f(g_mix)[0], f(g_ffn)[0], f(w_ffn_gate)[0],
                                   f(w_ffn_up)[0], f(w_ffn_down)[0], f(g_final))
    if L not in _NC_CACHE:
        _NC_CACHE[L] = build(L, upto=_UPTO)
    nc = _NC_CACHE[L]
    in_maps = [{"x": np.ascontiguousarray(x[b]), "wall": wall, "cst": cst, "biasT": biasT} for b in range(B)]
    res = run_bass_kernel_spmd(nc, in_maps, core_ids=list(range(B)))
    if DEBUG:
        global _DBG
        _DBG = res.results
    return np.stack([np.asarray(r["out"], np.float32) for r in res.results], axis=0)
```

```python
import math
import numpy as np
import ml_dtypes
from contextlib import ExitStack
import concourse.bass as bass
import concourse.mybir as mybir
from concourse.bass_utils import run_bass_kernel_spmd

F32 = mybir.dt.float32
BF16 = mybir.dt.bfloat16
U8 = mybir.dt.uint8
AF = mybir.ActivationFunctionType
ALU = mybir.AluOpType
AX = mybir.AxisListType

D_MODEL = 1024
D_CONV = 512
N_HEADS = 8
HEAD_DIM = 64
D_ATTN = 512
N_IDX = 8
IDX_DIM = 64
TOPK = 256
N_BUCKETS = 32
MAX_EXACT = 16
MAX_DISTANCE = 128
D_FF = 2816
NFC = D_FF // 128
EPS = 1e-6
PROJ_SIZES = (512, 512, 512, 512, 64, 64, 512, 64, 8, 1024, 1024)
OFFS = np.concatenate([[0], np.cumsum(PROJ_SIZES)]).astype(int)
O_CB, O_CC, O_CX, O_Q, O_K, O_V, O_IQ, O_IK, O_IW, O_GA, O_GB = [int(v) for v in OFFS[:-1]]
T = 512
NEG = -30000.0
KBIS = 21
IW_SCALE = float(N_IDX ** -0.5 * IDX_DIM ** -0.5)

C_ID = 0
C_CAUS = 128
C_GF = 256
C_BG = C_GF + 1024
C_GM = C_BG + 16
C_GFFN = C_GM + 8
C_CW = C_GFFN + 8
C_BFAR = C_CW + 12
C_P2 = C_BFAR + 8
C_ONE = C_P2 + KBIS
NCST = C_ONE + 64

E_K, E_IK, E_TM = 0, 1, 2
E_Q = 3
E_IQ = 7
E_CONV = 11
E_MIX = 23
E_OUT = 55
E_GU = 63
E_DN = 107
M_ENT = 131
GRP = 2
RING = 8


class Sched:
    def __init__(self, upto=99):
        self.ops = []
        self.upto = upto
        self.on = True

    def allkeys(self):
        ks = set()
        for op in self.ops:
            ks.update(op[2]); ks.update(op[3])
        return sorted(k for k in ks if not k.startswith("outd") and not k.startswith("wbf"))

    def stage(self, n):
        self.main_on = n <= self.upto
        self.on = self.main_on

    def sub(self, v):
        self.on = self.main_on and v <= SUBUPTO

    def add(self, eng, fn, r=(), w=(), dma=False):
        if self.on:
            self.ops.append((eng, fn, tuple(r), tuple(w), dma))

    def analyze(self, sems, dma_sems):
        ops = self.ops
        n = len(ops)
        last_w, readers = {}, {}
        deps = []
        for i, (eng, fn, r, w, dma) in enumerate(ops):
            d = set()
            for b in r:
                if b in last_w:
                    d.add(last_w[b])
                if b.startswith("ps"):
                    d.update(p for p in readers.get(b, ()) if ops[p][0] != eng)
            for b in w:
                if b in last_w:
                    d.add(last_w[b])
                d.update(readers.get(b, ()))
            d.discard(i)
            for b in r:
                readers.setdefault(b, []).append(i)
            for b in w:
                last_w[b] = i
                readers[b] = []
            deps.append(d)
        red = []
        marked = set()
        for i in range(n):
            eng = ops[i][0]
            best = {}
            dl = []
            for p in deps[i]:
                pe, _, _, _, pd = ops[p]
                if pd:
                    dl.append(p)
                    continue
                if pe == eng and eng == "tensor":
                    continue
                if pe not in best or best[pe] < p:
                    best[pe] = p
            lst = list(best.values()) + dl
            red.append(lst)
            marked.update(lst)
        cnt = {}
        self.inc = {}
        dcount = {}
        self.prewait = {}
        for i in range(n):
            eng, _, _, _, dma = ops[i]
            if dma:
                k = dcount.get(eng, 0)
                dcount[eng] = k + 1
                ns = len(dma_sems[eng])
                sem = dma_sems[eng][k % ns]
                self.inc[i] = (sem, 16 * (k // ns + 1))
                if k >= ns:
                    self.prewait[i] = (sem, 16 * (k // ns))
            elif i in marked:
                cnt[eng] = cnt.get(eng, 0) + 1
                self.inc[i] = (sems[eng], cnt[eng])
        self.waits = []
        for i in range(n):
            wl = [self.inc[p] for p in red[i]]
            if i in self.prewait:
                wl.append(self.prewait[i])
            self.waits.append(wl)
        self.by_eng = {}
        for i in range(n):
            self.by_eng.setdefault(ops[i][0], []).append(i)

    def emit(self, engname, e):
        waited = {}
        for i in self.by_eng.get(engname, []):
            eng, fn, r, w, dma = self.ops[i]
            for sem, val in self.waits[i]:
                k = id(sem)
                if waited.get(k, 0) < val:
                    e.wait_ge(sem, val)
                    waited[k] = val
            inst = fn(e)
            if i in self.inc:
                sem, val = self.inc[i]
                inst.then_inc(sem, 16 if dma else 1)

    def final_waits(self, e, engname):
        pass


def build(L, kbis=KBIS, upto=99):
    NT = L // T
    NKB = L // 128
    nc = bass.Bass("TRN2", target_bir_lowering=False)
    x_d = nc.dram_tensor("x", [L, D_MODEL], F32, kind="ExternalInput").ap()
    wall_d = nc.dram_tensor("wall", [M_ENT, 128, 1024], F32, kind="ExternalInput").ap()
    cst_d = nc.dram_tensor("cst", [128, NCST], F32, kind="ExternalInput").ap()
    bias_d = nc.dram_tensor("biasT", [128, 2048], F32, kind="ExternalInput").ap()
    out_d = nc.dram_tensor("out", [L, D_MODEL], F32, kind="ExternalOutput").ap()
    wbf_d = nc.dram_tensor("wbf", [M_ENT, 128, 1024], BF16).ap()
    if DEBUG:
        d_bis = nc.dram_tensor("d_bis", [128, 64], F32, kind="ExternalOutput").ap()
        d_sc = nc.dram_tensor("d_sc", [128, 4096], F32, kind="ExternalOutput").ap()
        d_mask = nc.dram_tensor("d_mask", [128, 4096], F32, kind="ExternalOutput").ap()
        d_attn = nc.dram_tensor("d_attn", [128, 2048], F32, kind="ExternalOutput").ap()

    S = Sched(upto)
    es = ExitStack()

    def sb(name, shape, dt):
        return es.enter_context(nc.sbuf_tensor("sb_" + name, shape, dt))

    with es:
        cst = sb("cst", [128, NCST], F32)
        identb = sb("identb", [128, 128], BF16)
        bhi = sb("bhi", [128, 2048], BF16)
        blo = sb("blo", [128, 2048], BF16)
        kT = sb("kT", [128, L], BF16)
        ikT = sb("ikT", [128, L], BF16)
        vab = sb("vab", [128, NKB, 192], BF16)
        uhalo = sb("uhalo", [128, 4, 2], F32)
        wtok = sb("wtok", [128, 4, 8], F32)
        xt = sb("xt", [128, 4, 1024], F32)
        hT = sb("hT", [128, 8, T], BF16)
        yaT = sb("yaT", [128, 4, T], BF16)
        qTe = sb("qTe", [128, 4, T], BF16)
        qTo = sb("qTo", [128, 4, T], BF16)
        iqTe = sb("iqTe", [128, 4, T], BF16)
        iqTo = sb("iqTo", [128, 4, T], BF16)
        attnT = sb("attnT", [128, 4, T], BF16)
        arA = sb("arA", [128, 16384], BF16)
        arB = sb("arB", [128, 8200], F32)
        maskb = sb("maskb", [128, 4096], BF16)
        arC = sb("arC", [128, 4096], BF16)
        dtl = sb("dtl", [128, 2, 8, 128], BF16)
        rl = sb("rl", [128, 4, T], BF16)
        ring = sb("ring", [128, RING, 1024], BF16)
        sm = sb("sm", [128, 64], F32)
        bis = sb("bis", [128, 2, 32], F32)
        steps = sb("steps", [128, 2, 32], F32)
        psb = [es.enter_context(nc.psum_tensor("ps%d" % i, [128, 512], F32)) for i in range(8)]

        negm = arA[:, 0:NKB * 512].rearrange("p (k t) -> p k t", t=512)
        act = arA[:, 0:NFC * 512].rearrange("p (k t) -> p k t", t=512)
        A_KEYS = tuple("A%d" % i for i in range(32))

        def negm_keys(kb):
            return ("A%d" % kb,)

        def act_keys(fc):
            return ("A%d" % fc,)

        scores = [arB[:, 0:4096], arB[:, 4096:8192]]
        merged = arB[:, 0:2048].bitcast(BF16).rearrange("p (k t) -> p k t", t=512)
        moT = [arB[:, 2048:2560], arB[:, 2560:3072]]
        tmpa = [arB[:, 3072:3584], arB[:, 3584:4096]]
        SC0_KEYS = tuple(["mg%d" % i for i in range(8)] + ["moT0", "moT1", "tmpa0", "tmpa1"])
        sg = [arB[:, 4096:4608], arB[:, 4608:5120]]
        t2 = [arB[:, 5120:5632], arB[:, 5632:6144]]
        den_sb = arB[:, 6144:6656]
        rbc = arB[:, 6656:7168]
        cct = arB[:, 7168:7680]
        ubuf = arB[:, 7680:7680 + 514]
        yt = arB[:, 4096:4608]
        SC1_KEYS = ("sg0", "sg1", "t20", "t21", "den", "rbc", "cct", "ubuf")
        SC_KEYS = [SC0_KEYS, SC1_KEYS]
        xn = [arC[:, 0:1024], arC[:, 1024:2048]]
        PT = [arC[:, 2048 + i * 512: 2048 + (i + 1) * 512] for i in range(4)] + \
             [arC[:, i * 512:(i + 1) * 512] for i in range(4)]
        NPT = 8

        def pt_keys(pi):
            return ("PT%d" % pi,) if pi < 4 else ("PT%d" % pi, "xn%d" % ((pi - 4) // 2))
        junk = arC[:, 0:4096]

        ident_f = cst[:, C_ID:C_ID + 128]
        caus = cst[:, C_CAUS:C_CAUS + 128]

        def bank(i):
            return psb[i][:]

        def bankb(i):
            return psb[i][:].bitcast(BF16)

        S.add("sync", lambda e: e.dma_start(out=cst[:], in_=cst_d), w=("cst",), dma=True)
        S.add("sync", lambda e: e.dma_start(out=arB[:, 0:2048], in_=bias_d), w=SC0_KEYS, dma=True)
        S.add("vector", lambda e: e.tensor_copy(out=identb[:], in_=ident_f), r=("cst",), w=("identb",))
        S.add("vector", lambda e: e.tensor_copy(out=bhi[:], in_=arB[:, 0:2048]), r=SC0_KEYS, w=("bhi",))
        S.add("vector", lambda e: e.tensor_tensor(out=arB[:, 2048:4096], in0=arB[:, 0:2048], in1=bhi[:],
                                                  op=ALU.subtract), r=SC0_KEYS + ("bhi",), w=SC0_KEYS)
        S.add("vector", lambda e: e.tensor_copy(out=blo[:], in_=arB[:, 2048:4096]), r=SC0_KEYS, w=("blo",))
        S.add("vector", lambda e: e.memset(vab[:], 0.0), w=("vab",))
        for zi, zt in enumerate((qTe, qTo, iqTe, iqTo)):
            S.add("vector" if zi % 2 == 0 else "gpsimd", (lambda zt: lambda e: e.memset(zt[:], 0.0))(zt), w=("zq%d" % zi,))
        S.add("vector", lambda e: e.memset(vab[:, :, 64:65], 1.0), w=("vab",))
        S.add("vector", lambda e: e.memset(uhalo[:], 0.0), w=("uhalo",))
        CH = 8
        for a in range(0, M_ENT, CH):
            b = min(M_ENT, a + CH)
            S.add("gpsimd", (lambda a, b: lambda e: e.dma_start(out=wbf_d[a:b], in_=wall_d[a:b]))(a, b),
                  w=tuple("wbf%d" % i for i in range(a, b)), dma=True)

        NGRP = (M_ENT + GRP - 1) // GRP
        TOTG = NGRP * NT
        NSLOTG = RING // GRP
        state = {"loaded": 0}

        def load_group(g):
            j, gi = divmod(g, NGRP)
            e0 = gi * GRP
            e1 = min(M_ENT, e0 + GRP)
            s0 = (g % NSLOTG) * GRP
            n = e1 - e0
            S.add("sync", lambda e: e.dma_start(
                out=ring[:, s0:s0 + n, :], in_=wbf_d[e0:e1].rearrange("e p f -> p e f")),
                r=tuple("wbf%d" % i for i in range(e0, e1)),
                w=tuple("ring%d" % (s0 + i) for i in range(n)), dma=True)

        def want(j, ent):
            g = j * NGRP + ent // GRP
            while S.on and state["loaded"] <= min(TOTG - 1, g + NSLOTG - 1):
                load_group(state["loaded"])
                state["loaded"] += 1
            slot = (g % NSLOTG) * GRP + ent % GRP
            return ring[:, slot, :].rearrange("p (k c) -> p k c", c=128), "ring%d" % slot

        ps_rot = {"i": 0}

        def next_bank(lo=0, hi=4):
            i = ps_rot["i"]
            ps_rot["i"] = (i + 1) % 4
            return i

        def mm(out, lhsT, rhs, start, stop, r, w):
            S.add("tensor", lambda e: e.matmul(out, lhsT, rhs, start=start, stop=stop), r=r, w=w)

        def proj_fm(j, ent, nk, rhs_fn, rkeys, b):
            wap, wkey = want(j, ent)
            for kc in range(nk):
                mm(bank(b), wap[:, kc, :], rhs_fn(kc), kc == 0, kc == nk - 1,
                   r=(wkey,) + tuple(rkeys(kc)), w=("ps%d" % b,))

        def rmsnorm_to_hT(gcol, tagr):
            for s in range(4):
                S.sub(1)
                S.add("scalar", (lambda s: lambda e: e.activation(
                    out=xn[s % 2], in_=xt[:, s, :], func=AF.Square, accum_out=sm[:, s:s + 1]))(s),
                    r=("xt%d" % s,), w=("xn%d" % (s % 2), "sm%d" % s))
                S.sub(2)
                S.add("vector", (lambda s: lambda e: e.tensor_scalar(
                    out=sm[:, 4 + s:5 + s], in0=sm[:, s:s + 1], scalar1=1.0 / D_MODEL, scalar2=EPS,
                    op0=ALU.mult, op1=ALU.add))(s), r=("sm%d" % s,), w=("sm%d" % (4 + s),))
                S.sub(3)
                S.add("scalar", (lambda s: lambda e: e.activation(
                    out=sm[:, 8 + s:9 + s], in_=sm[:, 4 + s:5 + s], func=AF.Sqrt))(s),
                    r=("sm%d" % (4 + s),), w=("sm%d" % (8 + s),))
                S.add("vector", (lambda s: lambda e: e.reciprocal(
                    out=sm[:, 12 + s:13 + s], in_=sm[:, 8 + s:9 + s]))(s),
                    r=("sm%d" % (8 + s),), w=("sm%d" % (12 + s),))
                S.sub(4)
                S.add("vector", (lambda s: lambda e: e.tensor_scalar(
                    out=xn[s % 2], in0=xt[:, s, :], scalar1=sm[:, 12 + s:13 + s], scalar2=None,
                    op0=ALU.mult))(s), r=("xt%d" % s, "sm%d" % (12 + s)), w=("xn%d" % (s % 2),))
                S.sub(5)
                b = 4 + (s % 2)
                for c in range(8):
                    S.add("tensor", (lambda s, c, b: lambda e: e.transpose(
                        out=bankb(b)[:, c * 128:(c + 1) * 128], in_=xn[s % 2][:, c * 128:(c + 1) * 128],
                        identity=identb[:]))(s, c, b),
                        r=("xn%d" % (s % 2), "identb"), w=("ps%d" % b,))
                S.sub(6)
                for c in range(8):
                    eng = "vector" if c % 2 == 0 else "gpsimd"
                    if eng == "gpsimd":
                        eng = EVAC_ENG
                    if eng == "vector":
                        S.add("vector", (lambda s, c, b: lambda e: e.tensor_scalar(
                            out=hT[:, c, s * 128:(s + 1) * 128], in0=bankb(b)[:, c * 128:(c + 1) * 128],
                            scalar1=cst[:, gcol + c:gcol + c + 1], scalar2=None, op0=ALU.mult))(s, c, b),
                            r=("ps%d" % b, "cst"), w=("hT%d" % c,))
                    else:
                        S.add("scalar", (lambda s, c, b: lambda e: e.activation(
                            out=hT[:, c, s * 128:(s + 1) * 128], in_=bankb(b)[:, c * 128:(c + 1) * 128],
                            func=AF.Identity, scale=cst[:, gcol + c:gcol + c + 1]))(s, c, b),
                            r=("ps%d" % b, "cst"), w=("hT%d" % c,))

            S.sub(0)

        XT_KEYS = ("xt0", "xt1", "xt2", "xt3")
        HT_KEYS = tuple("hT%d" % c for c in range(8))

        def hT_rhs(kc):
            return hT[:, kc, :]

        def hT_keys(kc):
            return ("hT%d" % kc,)

        if DEBUG3:
            for ent in range(min(M_ENT, L // 128)):
                wap, wkey = want(0, ent)
                slot = int(wkey[4:])
                S.add("vector", (lambda slot, ent: lambda e: e.tensor_copy(
                    out=arB[:, (ent % 2) * 1024:(ent % 2) * 1024 + 1024], in_=ring[:, slot, :]))(slot, ent),
                    r=(wkey,), w=("stg%d" % (ent % 2),))
                S.add("gpsimd", (lambda ent: lambda e: e.dma_start(
                    out=out_d[128 * ent:128 * ent + 128, :], in_=arB[:, (ent % 2) * 1024:(ent % 2) * 1024 + 1024]))(ent),
                    r=("stg%d" % (ent % 2),), w=("od%d" % ent,), dma=True)
            S.add("gpsimd", lambda e: e.nop(), r=tuple("od%d" % ent for ent in range(min(M_ENT, L // 128))))
            NT = 0
        for j in range(NT):
            t0 = j * T
            S.stage(1)
            for s_ in range(4):
                S.add("gpsimd", (lambda t0, s_: lambda e: e.dma_start(
                    out=xt[:, s_, :], in_=x_d[t0 + 128 * s_:t0 + 128 * s_ + 128, :]))(t0, s_),
                    w=("xt%d" % s_,), dma=True)
            rmsnorm_to_hT(C_GM, "a")

            S.stage(2)
            S.sub(1)
            b = next_bank()
            proj_fm(j, E_K, 8, hT_rhs, hT_keys, b)
            S.add("scalar", (lambda b, t0: lambda e: e.activation(out=kT[:, t0:t0 + T], in_=bank(b), func=AF.Copy))(b, t0),
                  r=("ps%d" % b,), w=tuple("kT%d" % (4 * j + i) for i in range(4)))
            S.sub(2)
            b = next_bank()
            proj_fm(j, E_IK, 8, hT_rhs, hT_keys, b)
            S.add("vector", (lambda b, t0: lambda e: e.tensor_copy(out=ikT[:, t0:t0 + T], in_=bank(b)))(b, t0),
                  r=("ps%d" % b,), w=tuple("ikT%d" % (4 * j + i) for i in range(4)))
            S.sub(3)
            b = next_bank()
            wap, wkey = want(j, E_TM)
            for s in range(4):
                for kc in range(8):
                    mm(bank(b)[:, s * 128:s * 128 + 72], hT[:, kc, s * 128:(s + 1) * 128], wap[:, kc, 0:72],
                       kc == 0, kc == 7, r=(wkey, "hT%d" % kc), w=("ps%d" % b,))
            for s in range(4):
                S.add("scalar", (lambda b, s, kbw: lambda e: e.activation(
                    out=vab[:, kbw, 0:64], in_=bank(b)[:, s * 128:s * 128 + 64], func=AF.Copy))(b, s, 4 * j + s),
                    r=("ps%d" % b, "vab"), w=("vab%d" % (4 * j + s),))
                S.add("vector", (lambda b, s, kbw: lambda e: e.tensor_copy(
                    out=vab[:, kbw, 128:192], in_=bank(b)[:, s * 128:s * 128 + 64]))(b, s, 4 * j + s),
                    r=("ps%d" % b, "vab"), w=("vab%d" % (4 * j + s),))
                S.add("vector", (lambda b, s: lambda e: e.tensor_scalar(
                    out=wtok[:, s, :], in0=bank(b)[:, s * 128 + 64:s * 128 + 72], scalar1=IW_SCALE,
                    scalar2=None, op0=ALU.mult))(b, s), r=("ps%d" % b,), w=("wtok%d" % s,))
            S.sub(4)
            for c in range(4):
                b = next_bank()
                proj_fm(j, E_Q + c, 8, hT_rhs, hT_keys, b)
                S.add("scalar", (lambda b, c: lambda e: e.activation(
                    out=qTe[0:64, c, :], in_=bank(b)[0:64, :], func=AF.Copy, scale=HEAD_DIM ** -0.5))(b, c),
                    r=("ps%d" % b, "zq0"), w=("qT%d" % c,))
                S.add("scalar", (lambda b, c: lambda e: e.activation(
                    out=qTo[64:128, c, :], in_=bank(b)[64:128, :], func=AF.Copy, scale=HEAD_DIM ** -0.5))(b, c),
                    r=("ps%d" % b, "zq1"), w=("qT%d" % c,))
            S.sub(5)
            for c in range(4):
                b = next_bank()
                proj_fm(j, E_IQ + c, 8, hT_rhs, hT_keys, b)
                S.add("vector", (lambda b, c: lambda e: e.tensor_copy(out=iqTe[0:64, c, :], in_=bank(b)[0:64, :]))(b, c),
                      r=("ps%d" % b, "zq2"), w=("iqT%d" % c,))
                S.add("vector", (lambda b, c: lambda e: e.tensor_copy(out=iqTo[64:128, c, :], in_=bank(b)[64:128, :]))(b, c),
                      r=("ps%d" % b, "zq3"), w=("iqT%d" % c,))
            S.sub(6)
            for c in range(4):
                b = next_bank()
                proj_fm(j, E_CONV + 3 * c, 8, hT_rhs, hT_keys, b)
                S.add("scalar", (lambda b: lambda e: e.activation(out=cct, in_=bank(b), func=AF.Copy))(b),
                      r=("ps%d" % b,), w=("cct",))
                b = next_bank()
                proj_fm(j, E_CONV + 3 * c + 1, 8, hT_rhs, hT_keys, b)
                S.add("gpsimd", (lambda c: lambda e: e.tensor_copy(out=ubuf[:, 0:2], in_=uhalo[:, c, :]))(c),
                      r=("uhalo",), w=("ubuf",))
                S.add("vector", (lambda b: lambda e: e.tensor_tensor(
                    out=ubuf[:, 2:514], in0=bank(b), in1=cct, op=ALU.mult))(b),
                    r=("ps%d" % b, "cct"), w=("ubuf",))
                S.add("gpsimd", (lambda c: lambda e: e.tensor_copy(out=uhalo[:, c, :], in_=ubuf[:, 512:514]))(c),
                      r=("ubuf",), w=("uhalo",))
                cw = C_CW + 3 * c
                S.add("vector", (lambda cw: lambda e: e.tensor_scalar(
                    out=yt, in0=ubuf[:, 2:514], scalar1=cst[:, cw + 2:cw + 3], scalar2=None, op0=ALU.mult))(cw),
                    r=("ubuf", "cst"), w=("sg0",))
                S.add("vector", (lambda cw: lambda e: e.scalar_tensor_tensor(
                    out=yt, in0=ubuf[:, 1:513], scalar=cst[:, cw + 1:cw + 2], in1=yt,
                    op0=ALU.mult, op1=ALU.add))(cw), r=("ubuf", "cst", "sg0"), w=("sg0",))
                S.add("vector", (lambda cw: lambda e: e.scalar_tensor_tensor(
                    out=yt, in0=ubuf[:, 0:512], scalar=cst[:, cw:cw + 1], in1=yt,
                    op0=ALU.mult, op1=ALU.add))(cw), r=("ubuf", "cst", "sg0"), w=("sg0",))
                b = next_bank()
                proj_fm(j, E_CONV + 3 * c + 2, 8, hT_rhs, hT_keys, b)
                S.add("vector", (lambda b, c: lambda e: e.tensor_tensor(
                    out=yaT[:, c, :], in0=bank(b), in1=yt, op=ALU.mult))(b, c),
                    r=("ps%d" % b, "sg0"), w=("yaT%d" % c,))

            S.stage(3)
            for pair in range(2):
                blks = [2 * pair, 2 * pair + 1]
                for slot, tl in enumerate(blks):
                    tb = 4 * j + tl
                    n_s = (tb + 1) * 128
                    sck = SC_KEYS[slot]
                    for h in range(8):
                        S.add("vector", (lambda slot, h, tl: lambda e: e.tensor_scalar(
                            out=dtl[:, slot, h, :], in0=identb[:], scalar1=wtok[:, tl, h:h + 1], scalar2=None,
                            op0=ALU.mult))(slot, h, tl), r=("identb", "wtok%d" % tl), w=("dtl%d_%d" % (slot, h),))
                    nsc = (n_s + 511) // 512
                    for sc in range(nsc):
                        wd = min(512, n_s - sc * 512)
                        bs = 6 + (sc % 2)
                        dbk = {}

                        def idx_s1(h):
                            c, hf = h // 2, h % 2
                            bd = next_bank()
                            dbk[h] = bd
                            S.add("tensor", (lambda bd, c, hf, tl, sc, wd: lambda e: e.matmul(
                                bank(bd)[:, 0:wd], (iqTe if hf == 0 else iqTo)[:, c, tl * 128:(tl + 1) * 128],
                                ikT[:, sc * 512:sc * 512 + wd], start=True, stop=True))(bd, c, hf, tl, sc, wd),
                                r=("iqT%d" % c,) + tuple("ikT%d" % (4 * sc + i) for i in range((wd + 127) // 128)),
                                w=("ps%d" % bd,))
                            ri = h % 4
                            if h % 2 == 0:
                                S.add("scalar", (lambda bd, ri, wd: lambda e: e.activation(
                                    out=rl[:, ri, 0:wd], in_=bank(bd)[:, 0:wd], func=AF.Relu))(bd, ri, wd),
                                    r=("ps%d" % bd,), w=("rl%d" % ri,))
                            else:
                                S.add("vector", (lambda bd, ri, wd: lambda e: e.tensor_scalar(
                                    out=rl[:, ri, 0:wd], in0=bank(bd)[:, 0:wd], scalar1=0.0, scalar2=None,
                                    op0=ALU.max))(bd, ri, wd), r=("ps%d" % bd,), w=("rl%d" % ri,))

                        def idx_s3(h):
                            ri = h % 4
                            S.add("tensor", (lambda bs, slot, h, ri, wd: lambda e: e.matmul(
                                bank(bs)[:, 0:wd], dtl[:, slot, h, :], rl[:, ri, 0:wd], start=(h == 0), stop=(h == 7)))(bs, slot, h, ri, wd),
                                r=("dtl%d_%d" % (slot, h), "rl%d" % ri), w=("ps%d" % bs,))

                        for hh in range(8 + 3):
                            if hh < 8:
                                idx_s1(hh)
                            if hh >= 3:
                                idx_s3(hh - 3)
                        S.add("vector" if sc % 2 == 0 else "scalar",
                              (lambda bs, slot, sc, wd: (lambda e: e.tensor_copy(
                                  out=scores[slot][:, sc * 512:sc * 512 + wd], in_=bank(bs)[:, 0:wd])) if sc % 2 == 0 else
                               (lambda e: e.activation(out=scores[slot][:, sc * 512:sc * 512 + wd],
                                                       in_=bank(bs)[:, 0:wd], func=AF.Copy)))(bs, slot, sc, wd),
                              r=("ps%d" % bs,), w=sck)
                    S.add("vector", (lambda slot, n_s: lambda e: e.tensor_reduce(
                        out=bis[:, slot, 0:1], in_=scores[slot][:, 0:n_s], op=ALU.max, axis=AX.X))(slot, n_s),
                        r=sck, w=("bis%d_0" % slot,))
                    S.add("vector", (lambda slot: lambda e: e.tensor_scalar(
                        out=bis[:, slot, 1:2], in0=bis[:, slot, 0:1], scalar1=-31.0, scalar2=None, op0=ALU.add))(slot),
                        r=sck + ("bis%d_0" % slot,), w=("bis%d_1" % slot,))
                    S.add("vector", (lambda slot: lambda e: e.tensor_scalar(
                        out=bis[:, slot, 1:2], in0=bis[:, slot, 1:2], scalar1=-1.0, scalar2=None, op0=ALU.add))(slot),
                        r=("bis%d_1" % slot,), w=("bis%d_1" % slot,))
                    S.add("gpsimd", (lambda slot, n_s: lambda e: e.tensor_tensor(
                        out=scores[slot][:, n_s - 128:n_s], in0=scores[slot][:, n_s - 128:n_s], in1=caus,
                        op=ALU.add))(slot, n_s), r=sck + ("cst", "bis%d_0" % slot, "bis%d_1" % slot), w=sck)
                    S.add("gpsimd", (lambda slot: lambda e: e.tensor_tensor(
                        out=bis[:, slot, 2:3], in0=bis[:, slot, 0:1], in1=bis[:, slot, 1:2], op=ALU.subtract))(slot),
                        r=("bis%d_0" % slot, "bis%d_1" % slot), w=("bis%d_2" % slot,))
                    S.add("gpsimd", (lambda slot: lambda e: e.tensor_scalar(
                        out=steps[:, slot, 0:kbis], in0=cst[:, C_P2:C_P2 + kbis], scalar1=bis[:, slot, 2:3],
                        scalar2=None, op0=ALU.mult))(slot), r=("cst", "bis%d_2" % slot), w=("steps%d" % slot,))
                    S.add("gpsimd", (lambda slot: lambda e: e.tensor_tensor(
                        out=bis[:, slot, 3:4], in0=bis[:, slot, 1:2], in1=steps[:, slot, 0:1], op=ALU.add))(slot),
                        r=("bis%d_1" % slot, "steps%d" % slot), w=("bis%d_3" % slot,))
                junkV = arC[:, 0:4096].bitcast(U8)[:, 0:4096]
                junkA = arC[:, 0:4096].bitcast(U8)[:, 4096:8192]
                for k in range(kbis):
                    for slot, tl in enumerate(blks):
                        n_s = (4 * j + tl + 1) * 128
                        if slot == 0:
                            S.add("vector", (lambda slot, n_s: lambda e: e.tensor_scalar(
                                out=junkV[:, 0:n_s], in0=scores[slot][:, 0:n_s], scalar1=bis[:, slot, 3:4], scalar2=None,
                                op0=ALU.is_ge, op1=ALU.add, accum_out=bis[:, slot, 4:5]))(slot, n_s),
                                r=SC_KEYS[slot] + ("bis%d_3" % slot,), w=("bis%d_4" % slot,))
                        else:
                            S.add("scalar", (lambda slot, n_s: lambda e: e.activation(
                                out=junkA[:, 0:n_s], in_=scores[slot][:, 0:n_s], func=AF.Sign, scale=-1.0,
                                bias=bis[:, slot, 3:4], accum_out=bis[:, slot, 4:5]))(slot, n_s),
                                r=SC_KEYS[slot] + ("bis%d_3" % slot,), w=("bis%d_4" % slot,))
                    for slot, tl in enumerate(blks):
                        n_s = (4 * j + tl + 1) * 128
                        if slot == 0:
                            S.add("gpsimd", (lambda slot: lambda e: e.tensor_scalar(
                                out=bis[:, slot, 5:6], in0=bis[:, slot, 4:5], scalar1=float(TOPK) - 0.5, scalar2=-0.5,
                                op0=ALU.is_ge, op1=ALU.add))(slot), r=("bis%d_4" % slot,), w=("bis%d_5" % slot,))
                        else:
                            S.add("gpsimd", (lambda slot, n_s: lambda e: e.tensor_scalar(
                                out=bis[:, slot, 5:6], in0=bis[:, slot, 4:5], scalar1=float(n_s - 2 * TOPK + 1), scalar2=-0.5,
                                op0=ALU.is_le, op1=ALU.add))(slot, n_s), r=("bis%d_4" % slot,), w=("bis%d_5" % slot,))
                        if k < kbis - 1:
                            S.add("gpsimd", (lambda slot, k: lambda e: e.tensor_scalar(
                                out=bis[:, slot, 3:4], in0=bis[:, slot, 5:6], scalar1=steps[:, slot, k:k + 1],
                                scalar2=bis[:, slot, 3:4], op0=ALU.mult, op1=ALU.add))(slot, k),
                                r=("bis%d_5" % slot, "steps%d" % slot, "bis%d_3" % slot), w=("bis%d_3" % slot,))
                        else:
                            S.add("gpsimd", (lambda slot: lambda e: e.tensor_scalar(
                                out=bis[:, slot, 5:6], in0=bis[:, slot, 5:6], scalar1=-0.5, scalar2=None,
                                op0=ALU.add))(slot), r=("bis%d_5" % slot,), w=("bis%d_5" % slot,))
                            S.add("gpsimd", (lambda slot, k: lambda e: e.tensor_scalar(
                                out=bis[:, slot, 3:4], in0=bis[:, slot, 5:6], scalar1=steps[:, slot, k:k + 1],
                                scalar2=bis[:, slot, 3:4], op0=ALU.mult, op1=ALU.add))(slot, k),
                                r=("bis%d_5" % slot, "steps%d" % slot, "bis%d_3" % slot), w=("bis%d_3" % slot,))
                for slot, tl in enumerate(blks):
                    tb = 4 * j + tl
                    n_s = (tb + 1) * 128
                    S.add("vector", (lambda slot, n_s: lambda e: e.tensor_scalar(
                        out=maskb[:, 0:n_s], in0=scores[slot][:, 0:n_s], scalar1=bis[:, slot, 3:4], scalar2=None,
                        op0=ALU.is_ge))(slot, n_s), r=SC_KEYS[slot] + ("bis%d_3" % slot,), w=("maskb",))
                    for g0 in range(0, tb + 1, 8):
                        g1 = min(tb + 1, g0 + 8)
                        bt = 4 + ((g0 // 8) % 2)
                        for kb in range(g0, g1):
                            S.add("tensor", (lambda bt, kb, g0: lambda e: e.transpose(
                                out=bankb(bt)[:, (kb - g0) * 128:(kb - g0 + 1) * 128],
                                in_=maskb[:, kb * 128:(kb + 1) * 128], identity=identb[:]))(bt, kb, g0),
                                r=("maskb", "identb"), w=("ps%d" % bt,))
                        ng = g1 - g0
                        eng = "vector" if (g0 // 8) % 2 == 0 else "gpsimd"
                        eng = "vector"
                        S.add(eng, (lambda bt, g0, ng, tl: lambda e: e.tensor_scalar(
                            out=negm[:, g0:g0 + ng, tl * 128:(tl + 1) * 128],
                            in0=bankb(bt)[:, 0:ng * 128].rearrange("p (k t) -> p k t", t=128),
                            scalar1=-1.0, scalar2=-NEG, op0=ALU.add, op1=ALU.mult))(bt, g0, ng, tl),
                            r=("ps%d" % bt,), w=tuple("A%d" % kb for kb in range(g0, g1)))

            S.stage(4)
            nkb_j = 4 * j + 4
            pend_norm = [None]
            for h in range(8):
                c, hf = h // 2, h % 2
                bacc = 6 + (h % 2)
                bden = 4 + (h % 2)
                def att_setup(kb):
                    a = kb - 4 * j
                    col0 = max(0, a) * 128
                    near = []
                    for which in (0, 1):
                        tl = kb + which - 4 * j
                        if 0 <= tl < 4:
                            near.append((tl, which))
                    return a, col0, near

                lgb = {}

                def att_s1(kb):
                    a, col0, near = att_setup(kb)
                    bl = next_bank()
                    lgb[kb] = bl
                    S.add("tensor", (lambda bl, hf, c, kb, col0: lambda e: e.matmul(
                        bank(bl)[:, col0:512], kT[:, kb * 128:(kb + 1) * 128],
                        (qTe if hf == 0 else qTo)[:, c, col0:512], start=True, stop=False))(bl, hf, c, kb, col0),
                        r=("kT%d" % kb, "qT%d" % c), w=("ps%d" % bl,))
                    if not NOMASK:
                      S.add("tensor", (lambda bl, kb, col0, last: lambda e: e.matmul(
                        bank(bl)[:, col0:512], identb[:], negm[:, kb, col0:512], start=False, stop=last))(bl, kb, col0, len(near) == 0),
                        r=("identb", "A%d" % kb), w=("ps%d" % bl,))
                    for ni, (tl, which) in enumerate(near):
                        bc0 = (h * 2 + which) * 128
                        for part, bt_ in enumerate((bhi, blo)):
                            last = (ni == len(near) - 1) and part == 1
                            S.add("tensor", (lambda bl, tl, bt_, bc0, last: lambda e: e.matmul(
                                bank(bl)[:, tl * 128:(tl + 1) * 128], identb[:], bt_[:, bc0:bc0 + 128],
                                start=False, stop=last))(bl, tl, bt_, bc0, last),
                                r=("identb", "bhi", "blo"), w=("ps%d" % bl,))

                def att_s2(kb):
                    a, col0, near = att_setup(kb)
                    bl = lgb[kb]
                    pi = kb % NPT
                    ncols = sorted(tl for tl, _ in near)
                    nlo = ncols[0] * 128 if ncols else 512
                    nhi = (ncols[-1] + 1) * 128 if ncols else 512
                    if ncols:
                        S.add("scalar", (lambda bl, pi, nlo, nhi: lambda e: e.activation(
                            out=PT[pi][:, nlo:nhi], in_=bank(bl)[:, nlo:nhi], func=AF.Exp))(bl, pi, nlo, nhi),
                            r=("ps%d" % bl,), w=pt_keys(pi))
                    if not ncols:
                        nhi = col0
                    if nhi < 512:
                        S.add("scalar", (lambda bl, pi, nhi, h: lambda e: e.activation(
                            out=PT[pi][:, nhi:512], in_=bank(bl)[:, nhi:512], func=AF.Exp,
                            bias=cst[:, C_BFAR + h:C_BFAR + h + 1]))(bl, pi, nhi, h),
                            r=("ps%d" % bl, "cst"), w=pt_keys(pi))

                def att_s3(kb):
                    a, col0, near = att_setup(kb)
                    pi = kb % NPT
                    vlo = 0 if hf == 0 else 64
                    S.add("tensor", (lambda bacc, kb, pi, col0, vlo, lastkb: lambda e: e.matmul(
                        bank(bacc)[:, col0:512], vab[:, kb, vlo:vlo + 128], PT[pi][:, col0:512],
                        start=(kb == 0), stop=lastkb))(bacc, kb, pi, col0, vlo, kb == nkb_j - 1),
                        r=("vab%d" % kb, "vab") + pt_keys(pi), w=("ps%d" % bacc,))

                def make_norm(bacc, bden, hf, c):
                    def norm():
                        dp = 64 if hf == 0 else 0
                        op0 = 0 if hf == 0 else 64
                        S.add("vector", (lambda bacc, dp: lambda e: e.tensor_copy(
                            out=den_sb[dp:dp + 1, :], in_=bank(bacc)[dp:dp + 1, :]))(bacc, dp),
                            r=("ps%d" % bacc,), w=("den",))
                        S.add("vector", (lambda dp: lambda e: e.reciprocal(out=den_sb[dp:dp + 1, :], in_=den_sb[dp:dp + 1, :]))(dp),
                              r=("den",), w=("den",))
                        S.add("tensor", (lambda bden, dp, op0: lambda e: e.matmul(
                            bank(bden)[op0:op0 + 64, :], cst[dp:dp + 1, C_ONE:C_ONE + 64], den_sb[dp:dp + 1, :],
                            start=True, stop=True))(bden, dp, op0), r=("cst", "den"), w=("ps%d" % bden,))
                        S.add("scalar", (lambda bden, op0: lambda e: e.activation(
                            out=rbc[op0:op0 + 64, :], in_=bank(bden)[op0:op0 + 64, :], func=AF.Copy))(bden, op0),
                            r=("ps%d" % bden,), w=("rbc",))
                        S.add("vector", (lambda bacc, op0, c: lambda e: e.tensor_tensor(
                            out=attnT[op0:op0 + 64, c, :], in0=bank(bacc)[op0:op0 + 64, :], in1=rbc[op0:op0 + 64, :],
                            op=ALU.mult))(bacc, op0, c), r=("ps%d" % bacc, "rbc"), w=("attnT%d_%d" % (c, hf),))
                    return norm

                SK = 3
                for kk in range(nkb_j + SK):
                    if kk < nkb_j:
                        att_s1(kk)
                        att_s2(kk)
                    if kk >= SK:
                        att_s3(kk - SK)
                    if kk == 2 and pend_norm[0] is not None:
                        pend_norm[0]()
                        pend_norm[0] = None
                if pend_norm[0] is not None:
                    pend_norm[0]()
                pend_norm[0] = make_norm(bacc, bden, hf, c)
            if pend_norm[0] is not None:
                pend_norm[0]()
                pend_norm[0] = None

            S.stage(5)
            for dc in range(8):
                e0 = E_MIX + 4 * dc
                ba = next_bank()
                proj_fm(j, e0, 4, lambda kc: yaT[:, kc, :], lambda kc: ("yaT%d" % kc,), ba)
                bg = next_bank()
                proj_fm(j, e0 + 1, 8, hT_rhs, hT_keys, bg)
                i2 = dc % 2
                S.add("scalar", (lambda bg, i2, dc: lambda e: e.activation(
                    out=sg[i2], in_=bank(bg), func=AF.Sigmoid, bias=cst[:, C_BG + dc:C_BG + dc + 1]))(bg, i2, dc),
                    r=("ps%d" % bg, "cst"), w=("sg%d" % i2,))
                S.add("vector", (lambda ba, i2: lambda e: e.tensor_tensor(
                    out=tmpa[i2], in0=bank(ba), in1=sg[i2], op=ALU.mult))(ba, i2),
                    r=("ps%d" % ba, "sg%d" % i2), w=("tmpa%d" % i2,))
                bb_ = next_bank()
                proj_fm(j, e0 + 2, 4, lambda kc: attnT[:, kc, :],
                        lambda kc: ("attnT%d_0" % kc, "attnT%d_1" % kc), bb_)
                bg2 = next_bank()
                proj_fm(j, e0 + 3, 8, hT_rhs, hT_keys, bg2)
                S.add("scalar", (lambda bg2, i2, dc: lambda e: e.activation(
                    out=t2[i2], in_=bank(bg2), func=AF.Sigmoid, bias=cst[:, C_BG + 8 + dc:C_BG + 9 + dc]))(bg2, i2, dc),
                    r=("ps%d" % bg2, "cst"), w=("t2%d" % i2,))
                S.add("vector", (lambda bb_, i2: lambda e: e.tensor_tensor(
                    out=t2[i2], in0=bank(bb_), in1=t2[i2], op=ALU.mult))(bb_, i2),
                    r=("ps%d" % bb_, "t2%d" % i2), w=("t2%d" % i2,))
                S.add("gpsimd", (lambda i2, dc: lambda e: e.tensor_tensor(
                    out=merged[:, dc, :], in0=t2[i2], in1=tmpa[i2], op=ALU.add))(i2, dc),
                    r=("t2%d" % i2, "tmpa%d" % i2), w=("mg%d" % dc,))

            def resid_a(bo, dc, i2):
                S.add("scalar", (lambda bo, i2: lambda e: e.activation(out=moT[i2], in_=bank(bo), func=AF.Copy))(bo, i2),
                      r=("ps%d" % bo,), w=("moT%d" % i2,))

            def resid_b(dc, i2):
                bt = 4 + (dc % 2)
                for s in range(4):
                    S.add("tensor", (lambda bt, s, i2: lambda e: e.transpose(
                        out=bank(bt)[:, s * 128:(s + 1) * 128], in_=moT[i2][:, s * 128:(s + 1) * 128],
                        identity=ident_f))(bt, s, i2), r=("moT%d" % i2, "cst"), w=("ps%d" % bt,))
                S.add("vector", (lambda bt, dc: lambda e: e.tensor_tensor(
                    out=xt[:, :, dc * 128:(dc + 1) * 128],
                    in0=bank(bt).rearrange("p (s d) -> p s d", d=128),
                    in1=xt[:, :, dc * 128:(dc + 1) * 128], op=ALU.add))(bt, dc),
                    r=("ps%d" % bt,) + XT_KEYS, w=XT_KEYS)

            for dc in range(8):
                bo = next_bank()
                proj_fm(j, E_OUT + dc, 8, lambda kc: merged[:, kc, :], lambda kc: ("mg%d" % kc,), bo)
                resid_a(bo, dc, dc % 2)
                if dc >= 1:
                    resid_b(dc - 1, (dc - 1) % 2)
            resid_b(7, 1)

            S.stage(6)
            rmsnorm_to_hT(C_GFFN, "b")
            for fc in range(NFC):
                bg = next_bank()
                proj_fm(j, E_GU + 2 * fc, 8, hT_rhs, hT_keys, bg)
                bu = next_bank()
                proj_fm(j, E_GU + 2 * fc + 1, 8, hT_rhs, hT_keys, bu)
                i2 = fc % 2
                S.add("scalar", (lambda bg, i2: lambda e: e.activation(out=sg[i2], in_=bank(bg), func=AF.Silu))(bg, i2),
                      r=("ps%d" % bg,), w=("sg%d" % i2,))
                S.add("vector", (lambda bu, i2, fc: lambda e: e.tensor_tensor(
                    out=act[:, fc, :], in0=bank(bu), in1=sg[i2], op=ALU.mult))(bu, i2, fc),
                    r=("ps%d" % bu, "sg%d" % i2), w=("A%d" % fc,))
            for dc in range(8):
                bo = next_bank()
                for part in range(3):
                    wap, wkey = want(j, E_DN + 3 * dc + part)
                    nk = 8 if part < 2 else NFC - 16
                    for ko in range(nk):
                        fo = part * 8 + ko
                        mm(bank(bo), wap[:, ko, :], act[:, fo, :], fo == 0, fo == NFC - 1,
                           r=(wkey, "A%d" % fo), w=("ps%d" % bo,))
                resid_a(bo, dc, dc % 2)
                if dc >= 1:
                    resid_b(dc - 1, (dc - 1) % 2)
            resid_b(7, 1)
            S.stage(1)
            for s in range(4):
                S.add("scalar", (lambda s: lambda e: e.activation(
                    out=xn[s % 2], in_=xt[:, s, :], func=AF.Square, accum_out=sm[:, s:s + 1]))(s),
                    r=("xt%d" % s,), w=("xn%d" % (s % 2), "sm%d" % s))
                S.add("vector", (lambda s: lambda e: e.tensor_scalar(
                    out=sm[:, 4 + s:5 + s], in0=sm[:, s:s + 1], scalar1=1.0 / D_MODEL, scalar2=EPS,
                    op0=ALU.mult, op1=ALU.add))(s), r=("sm%d" % s,), w=("sm%d" % (4 + s),))
                S.add("scalar", (lambda s: lambda e: e.activation(
                    out=sm[:, 8 + s:9 + s], in_=sm[:, 4 + s:5 + s], func=AF.Sqrt))(s),
                    r=("sm%d" % (4 + s),), w=("sm%d" % (8 + s),))
                S.add("vector", (lambda s: lambda e: e.reciprocal(
                    out=sm[:, 12 + s:13 + s], in_=sm[:, 8 + s:9 + s]))(s),
                    r=("sm%d" % (8 + s),), w=("sm%d" % (12 + s),))
                S.add("vector", (lambda s: lambda e: e.scalar_tensor_tensor(
                    out=xt[:, s, :], in0=xt[:, s, :], scalar=sm[:, 12 + s:13 + s], in1=cst[:, C_GF:C_GF + 1024],
                    op0=ALU.mult, op1=ALU.mult))(s), r=("xt%d" % s, "sm%d" % (12 + s), "cst"), w=("xt%d" % s,))
            for s_ in range(4):
                S.add("gpsimd", (lambda t0, s_: lambda e: e.dma_start(
                    out=out_d[t0 + 128 * s_:t0 + 128 * s_ + 128, :], in_=xt[:, s_, :]))(t0, s_),
                    r=("xt%d" % s_,), w=("outd%d_%d" % (j, s_),), dma=True)

        S.stage(0)
        if DEBUG2:
            lastk = ("outd%d" % (NT - 1),)
            allk = tuple(S.allkeys())
            if DBGSET == 0:
                items = [
                    [(arB[:, 4096:5120], 1024)],
                    [(iqT[:, 0, :], 512), (iqT[:, 1, :], 512)],
                    [(iqT[:, 2, :], 512), (iqT[:, 3, :], 512)],
                    [(ikT[:, 0:1024], 1024)],
                    [(wtok[:].rearrange("p a b -> p (a b)"), 32), (bis[:].rearrange("p a b -> p (a b)"), 64),
                     (dtl[:, 1, 0, :], 128), (dtl[:, 1, 1, :], 128), (rl[:, 3, :], 512)],
                    [(maskb[:, 0:1024], 1024)],
                ]
                stg = arA[:, 0:16384].bitcast(F32)
                stgk = A_KEYS
            else:
                items = [
                    [(attnT[:, 0, :], 512), (attnT[:, 1, :], 512)],
                    [(attnT[:, 2, :], 512), (attnT[:, 3, :], 512)],
                    [(negm[:, 7, :], 512), (negm[:, 0, :], 512)],
                    [(negm[:, 4, :], 512), (negm[:, 3, :], 512)],
                    [(kT[:, 0:1024], 1024)],
                    [(qT[:, 0, :], 512), (qT[:, 1, :], 512)],
                ]
                if DBGSET == 2:
                    items = [
                        [(PT[0], 512), (PT[1], 512)],
                        [(PT[2], 512), (PT[3], 512)],
                        [(bank(7), 512), (bank(5), 512)],
                        [(attnT[:, 3, :], 512), (rbc, 512)],
                        [(bank(6), 512), (bank(4), 512)],
                        [(qT[:, 3, :], 512), (kT[:, 512:1024], 512)],
                    ]
                if DBGSET == 3:
                    vflat = vab[:].rearrange("p a b -> p (a b)")
                    items = [[(vflat[:, 0:1024], 1024)], [(vflat[:, 1024:1536], 512)]]
                stg = arB[:, 0:6144] if DBGSET == 1 else arA[:, 0:16384].bitcast(F32)
                stgk = SC0_KEYS + SC1_KEYS if DBGSET == 1 else A_KEYS
            for k, lst in enumerate(items):
                c0 = 0
                for ap, n in lst:
                    S.add("vector", (lambda ap, n, k, c0: lambda e: e.tensor_copy(
                        out=stg[:, k * 1024 + c0:k * 1024 + c0 + n], in_=ap))(ap, n, k, c0),
                        r=allk, w=stgk)
                    c0 += n
                S.add("sync", (lambda k: lambda e: e.dma_start(
                    out=out_d[128 * k:128 * k + 128, :], in_=stg[:, k * 1024:(k + 1) * 1024]))(k),
                    r=stgk + lastk, w=("outd0", "outd1"), dma=True)
        if DEBUG:
            S.add("gpsimd", lambda e: e.dma_start(out=d_bis, in_=bis[:].rearrange("p a b -> p (a b)")),
                  r=tuple("bis1_%d" % i for i in range(6)), w=("dd0",), dma=True)
            S.add("gpsimd", lambda e: e.dma_start(out=d_sc, in_=arB[:, 4096:8192]), r=SC1_KEYS, w=("dd1",), dma=True)
            S.add("gpsimd", lambda e: e.dma_start(out=d_mask, in_=maskb[:], max_dma_last_dim=1024), r=("maskb",), w=("dd2",), dma=True)
            S.add("gpsimd", lambda e: e.dma_start(out=d_attn, in_=attnT[:].rearrange("p a b -> p (a b)"), max_dma_last_dim=1024),
                  r=tuple("attnT%d_%d" % (c, hf) for c in range(4) for hf in range(2)), w=("dd3",), dma=True)
            S.add("gpsimd", lambda e: e.nop(), r=("dd0", "dd1", "dd2", "dd3"))
        for j in range(NT):
            pass
        S.add("gpsimd", lambda e: e.nop(), r=tuple("outd%d_%d" % (j, s_) for j in range(NT) for s_ in range(4)))

        sem_names = ["sync", "tensor", "vector", "scalar", "gpsimd"]
        sems = {n: es.enter_context(nc.semaphore("s_" + n)) for n in sem_names}
        dma_sems = {
            "sync": [es.enter_context(nc.semaphore("ds%d" % i)) for i in range(8)],
            "gpsimd": [es.enter_context(nc.semaphore("dg%d" % i)) for i in range(8)],
        }
        S.analyze(sems, dma_sems)
        with nc.Block() as block:
            @block.sync
            def _(e):
                S.emit("sync", e)

            @block.tensor
            def _(e):
                S.emit("tensor", e)

            @block.vector
            def _(e):
                S.emit("vector", e)

            @block.scalar
            def _(e):
                S.emit("scalar", e)

            @block.gpsimd
            def _(e):
                S.emit("gpsimd", e)
    return nc


def _rel_bucket(n):
    n = np.maximum(n, 0)
    large = MAX_EXACT + (np.log(np.maximum(n, 1).astype(np.float32) / MAX_EXACT)
                         / math.log(MAX_DISTANCE / MAX_EXACT) * (N_BUCKETS - MAX_EXACT)).astype(np.int32)
    large = np.minimum(large, N_BUCKETS - 1)
    return np.where(n < MAX_EXACT, n, large)


def _entry(wcols, nk):
    ncol = wcols.shape[1]
    e = np.zeros((128, 8, 128), np.float32)
    e[:, :nk, :ncol] = wcols.reshape(nk, 128, ncol).transpose(1, 0, 2)
    return e.reshape(128, 1024)


def host_layout(w_in, b_gate, conv_w, w_a, w_b, w_out, rel_bias, g_mix, g_ffn, w_g, w_u, w_d, g_final, kbis=KBIS):
    wall = np.zeros((M_ENT, 128, 1024), np.float32)
    W = w_in

    def cols(o, n):
        return W[:, o:o + n]

    wall[E_K] = _entry(np.concatenate([cols(O_K, 64), cols(O_K, 64)], axis=1), 8)
    wall[E_IK] = _entry(np.concatenate([cols(O_IK, 64), cols(O_IK, 64)], axis=1), 8)
    wall[E_TM] = _entry(np.concatenate([cols(O_V, 64), cols(O_IW, 8)], axis=1), 8)
    for c in range(4):
        wall[E_Q + c] = _entry(cols(O_Q + 128 * c, 128), 8)
        wall[E_IQ + c] = _entry(cols(O_IQ + 128 * c, 128), 8)
        wall[E_CONV + 3 * c] = _entry(cols(O_CC + 128 * c, 128), 8)
        wall[E_CONV + 3 * c + 1] = _entry(cols(O_CX + 128 * c, 128), 8)
        wall[E_CONV + 3 * c + 2] = _entry(cols(O_CB + 128 * c, 128), 8)
    for dc in range(8):
        sl = slice(128 * dc, 128 * dc + 128)
        wall[E_MIX + 4 * dc] = _entry(w_a[:, sl], 4)
        wall[E_MIX + 4 * dc + 1] = _entry(cols(O_GA + 128 * dc, 128), 8)
        wall[E_MIX + 4 * dc + 2] = _entry(w_b[:, sl], 4)
        wall[E_MIX + 4 * dc + 3] = _entry(cols(O_GB + 128 * dc, 128), 8)
        wall[E_OUT + dc] = _entry(w_out[:, sl], 8)
        wall[E_DN + 3 * dc] = _entry(w_d[0:1024, sl], 8)
        wall[E_DN + 3 * dc + 1] = _entry(w_d[1024:2048, sl], 8)
        wall[E_DN + 3 * dc + 2] = _entry(w_d[2048:2816, sl], 6)
    for fc in range(NFC):
        sl = slice(128 * fc, 128 * fc + 128)
        wall[E_GU + 2 * fc] = _entry(w_g[:, sl], 8)
        wall[E_GU + 2 * fc + 1] = _entry(w_u[:, sl], 8)

    cst = np.zeros((128, NCST), np.float32)
    cst[:, C_ID:C_ID + 128] = np.eye(128, dtype=np.float32)
    tt, ss = np.meshgrid(np.arange(128), np.arange(128), indexing="ij")
    cst[:, C_CAUS:C_CAUS + 128] = np.where(ss <= tt, 0.0, -1e30).astype(np.float32)
    cst[:, C_GF:C_GF + 1024] = g_final[None, :]
    cst[:, C_BG:C_BG + 16] = b_gate.reshape(16, 128).T
    cst[:, C_GM:C_GM + 8] = g_mix.reshape(8, 128).T
    cst[:, C_GFFN:C_GFFN + 8] = g_ffn.reshape(8, 128).T
    for c in range(4):
        for k in range(3):
            cst[:, C_CW + 3 * c + k] = conv_w[k, 128 * c:128 * c + 128]
    cst[:, C_BFAR:C_BFAR + 8] = rel_bias[N_BUCKETS - 1][None, :]
    cst[:, C_P2:C_P2 + kbis] = (0.5 ** np.arange(1, kbis + 1)).astype(np.float32)[None, :]
    cst[:, C_ONE:C_ONE + 64] = 1.0
    sl_, tl_ = np.meshgrid(np.arange(128), np.arange(128), indexing="ij")
    biasT = np.zeros((128, 8, 2, 128), np.float32)
    for which in (0, 1):
        bk = _rel_bucket(tl_ - sl_ + 128 * which)
        biasT[:, :, which, :] = rel_bias[bk].transpose(0, 2, 1)
    return wall, cst, biasT.reshape(128, 2048)


_NC_CACHE = {}
_UPTO = 99
SUBUPTO = 99
DEBUG = False
DEBUG2 = False
DEBUG3 = False
DBGSET = 0
NOMASK = False
EVAC_ENG = 'vector'


def kernel(x, g_mix, w_in, b_gate, conv_w, w_branch_a, w_branch_b, w_out, rel_bias,
           g_ffn, w_ffn_gate, w_ffn_up, w_ffn_down, g_final):
    x = np.asarray(x, np.float32)
    B, L, _ = x.shape
    f = lambda a: np.asarray(a, np.float32)
    wall, cst, biasT = host_layout(f(w_in)[0], f(b_gate)[0], f(conv_w)[0], f(w_branch_a)[0], f(w_branch_b)[0],
                                   f(w_out)[0], f(rel_bias), f(g_mix)[0], f(g_ffn)[0], f(w_ffn_gate)[0],
                                   f(w_ffn_up)[0], f(w_ffn_down)[0], f(g_final))
    if L not in _NC_CACHE:
        _NC_CACHE[L] = build(L, upto=_UPTO)
    nc = _NC_CACHE[L]
    in_maps = [{"x": np.ascontiguousarray(x[b]), "wall": wall, "cst": cst, "biasT": biasT} for b in range(B)]
    res = run_bass_kernel_spmd(nc, in_maps, core_ids=list(range(B)))
    if DEBUG:
        global _DBG
        _DBG = res.results
    return np.stack([np.asarray(r["out"], np.float32) for r in res.results], axis=0)
```
